# Optimizing a Trainium2 kernel written in Bass

```python
import jax, jax.numpy as jnp
from jax import lax
import numpy as np

D_MODEL = 1024
BATCH = 8
SEQ = 8192
DEPTH = 2

HEAD_DIM = 64
POOL_WIDTH = D_MODEL // 4
POOL_WINDOWS = (2, 4, 8, 16)
N_POOL_GROUPS = len(POOL_WINDOWS)
POOL_GROUP_DIM = POOL_WIDTH // N_POOL_GROUPS
CONV_WIDTH = (3 * D_MODEL) // 8
CONV_KERNEL = 31
SGU_WIDTH = D_MODEL - POOL_WIDTH - CONV_WIDTH
SGU_HEADS = SGU_WIDTH // HEAD_DIM
CHUNK = 128
D_MIX = POOL_WIDTH + CONV_WIDTH + SGU_WIDTH
D_IN = POOL_WIDTH + 2 * CONV_WIDTH + 2 * SGU_WIDTH
D_FF = ((8 * D_MODEL // 3 + 127) // 128) * 128
N_EXPERTS = 8
TOP_K = 2
D_FF_EXPERT = 7 * D_MODEL // 2
N_MOD = 6
EPS = 1e-6

kernel_name = "hybrid_pool_conv_sgu_moe_encoder"


def rms_norm(x, g):
    xf = x.astype(jnp.float32)
    y = xf * lax.rsqrt(jnp.mean(xf * xf, axis=-1, keepdims=True) + EPS)
    return (y * g.astype(jnp.float32)).astype(x.dtype)


def layer_norm(x, g, b):
    xf = x.astype(jnp.float32)
    mu = jnp.mean(xf, axis=-1, keepdims=True)
    var = jnp.mean(jnp.square(xf - mu), axis=-1, keepdims=True)
    y = (xf - mu) * lax.rsqrt(var + EPS) * g.astype(jnp.float32) + b.astype(jnp.float32)
    return y.astype(x.dtype)


def multiscale_pool(a, pool_w, pool_scale):
    S = a.shape[1]
    af = a.astype(jnp.float32)
    cs = jnp.pad(jnp.cumsum(af, axis=1), ((0, 0), (1, 0), (0, 0)))
    t = jnp.arange(S)
    outs = []
    for g, w in enumerate(POOL_WINDOWS):
        half = w // 2
        hi = jnp.clip(t + half, 0, S)
        lo = jnp.clip(t - half, 0, S)
        sl = slice(g * POOL_GROUP_DIM, (g + 1) * POOL_GROUP_DIM)
        cs_g = cs[:, :, sl]
        win_sum = jnp.take(cs_g, hi, axis=1) - jnp.take(cs_g, lo, axis=1)
        count = (hi - lo).astype(jnp.float32)[None, :, None]
        diff = (win_sum / count - af[:, :, sl]).astype(a.dtype)
        outs.append(jnp.einsum('bsc,cd->bsd', diff, pool_w[g]))
    return jnp.concatenate(outs, axis=-1) * pool_scale


def conformer_conv(p, conv_w, conv_b, ln_g, ln_b):
    val, gate = jnp.split(p, 2, axis=-1)
    glu = val * jax.nn.sigmoid(gate)
    y = lax.conv_general_dilated(
        glu, conv_w[:, None, :], window_strides=(1,),
        padding=[(CONV_KERNEL // 2, CONV_KERNEL // 2)],
        dimension_numbers=('NWC', 'WIO', 'NWC'),
        feature_group_count=CONV_WIDTH) + conv_b
    return jax.nn.silu(layer_norm(y, ln_g, ln_b))


def spatial_gating(p, ln_g, ln_b, sgu_w, sgu_b):
    B, S, _ = p.shape
    u, v = jnp.split(p, 2, axis=-1)
    v = layer_norm(v, ln_g, ln_b).reshape(B, S // CHUNK, CHUNK, SGU_HEADS, HEAD_DIM)
    mixed = jnp.einsum('hpq,bnqhd->bnphd', sgu_w, v) + sgu_b.T[:, :, None]
    return u * mixed.reshape(B, S, SGU_WIDTH)


def swiglu(h, w_gate, w_up, w_down):
    return (jax.nn.silu(h @ w_gate) * (h @ w_up)) @ w_down


def moe_swiglu(h, router_w, router_b, w_gate, w_up, w_down):
    logits = jnp.einsum('bsd,de->bse', h.astype(jnp.float32), router_w.astype(jnp.float32)) \
        + router_b.astype(jnp.float32)
    top_vals, top_idx = lax.top_k(logits, TOP_K)
    top_w = jax.nn.softmax(top_vals, axis=-1)
    combine = jnp.sum(jax.nn.one_hot(top_idx, N_EXPERTS, dtype=jnp.float32) * top_w[..., None], axis=-2)
    combine = combine.astype(h.dtype)
    y = jnp.zeros_like(h)
    for e in range(N_EXPERTS):
        y = y + combine[..., e:e + 1] * swiglu(h, w_gate[e], w_up[e], w_down[e])
    return y


def setup_inputs(seed: int = 0) -> dict:
    key = jax.random.key(seed)
    ks = jax.random.split(key, 32)
    n_dense = (DEPTH + 1) // 2
    n_moe = DEPTH // 2

    def nrm(k, shape, fan_in):
        return jax.random.normal(k, shape, jnp.float32) * (fan_in ** -0.5)

    def near_one(k, shape):
        return 1.0 + 0.1 * jax.random.normal(k, shape, jnp.float32)

    def small(k, shape):
        return 0.02 * jax.random.normal(k, shape, jnp.float32)

    return {
        "x": jax.random.normal(ks[0], (BATCH, SEQ, D_MODEL), jnp.float32),
        "c": jax.random.normal(ks[1], (BATCH, D_MODEL), jnp.float32),
        "ada_w": nrm(ks[2], (DEPTH, D_MODEL, N_MOD * D_MODEL), D_MODEL),
        "ada_b": small(ks[3], (DEPTH, N_MOD * D_MODEL)),
        "mix_norm_g": near_one(ks[4], (DEPTH, D_MODEL)),
        "ffn_norm_g": near_one(ks[5], (DEPTH, D_MODEL)),
        "w_in": nrm(ks[6], (DEPTH, D_MODEL, D_IN), D_MODEL),
        "pool_w": nrm(ks[7], (DEPTH, N_POOL_GROUPS, POOL_GROUP_DIM, POOL_GROUP_DIM), POOL_GROUP_DIM),
        "pool_scale": near_one(ks[8], (DEPTH, POOL_WIDTH)),
        "conv_w": nrm(ks[9], (DEPTH, CONV_KERNEL, CONV_WIDTH), CONV_KERNEL),
        "conv_b": small(ks[10], (DEPTH, CONV_WIDTH)),
        "conv_ln_g": near_one(ks[11], (DEPTH, CONV_WIDTH)),
        "conv_ln_b": small(ks[12], (DEPTH, CONV_WIDTH)),
        "sgu_ln_g": near_one(ks[13], (DEPTH, SGU_WIDTH)),
        "sgu_ln_b": small(ks[14], (DEPTH, SGU_WIDTH)),
        "sgu_w": nrm(ks[15], (DEPTH, SGU_HEADS, CHUNK, CHUNK), CHUNK),
        "sgu_b": near_one(ks[16], (DEPTH, SGU_HEADS, CHUNK)),
        "w_out": nrm(ks[17], (DEPTH, D_MIX, D_MODEL), D_MIX),
        "ffn_w_gate": nrm(ks[18], (n_dense, D_MODEL, D_FF), D_MODEL),
        "ffn_w_up": nrm(ks[19], (n_dense, D_MODEL, D_FF), D_MODEL),
        "ffn_w_down": nrm(ks[20], (n_dense, D_FF, D_MODEL), D_FF),
        "router_w": nrm(ks[21], (n_moe, D_MODEL, N_EXPERTS), D_MODEL),
        "router_b": 0.01 * jax.random.normal(ks[22], (n_moe, N_EXPERTS), jnp.float32),
        "moe_w_gate": nrm(ks[23], (n_moe, N_EXPERTS, D_MODEL, D_FF_EXPERT), D_MODEL),
        "moe_w_up": nrm(ks[24], (n_moe, N_EXPERTS, D_MODEL, D_FF_EXPERT), D_MODEL),
        "moe_w_down": nrm(ks[25], (n_moe, N_EXPERTS, D_FF_EXPERT, D_MODEL), D_FF_EXPERT),
        "final_norm_g": near_one(ks[26], (D_MODEL,)),
    }


def reference(x, c, ada_w, ada_b, mix_norm_g, ffn_norm_g, w_in, pool_w, pool_scale,
              conv_w, conv_b, conv_ln_g, conv_ln_b, sgu_ln_g, sgu_ln_b, sgu_w, sgu_b, w_out,
              ffn_w_gate, ffn_w_up, ffn_w_down, router_w, router_b,
              moe_w_gate, moe_w_up, moe_w_down, final_norm_g):
    cond = jax.nn.silu(c)
    for l in range(DEPTH):
        mod = cond @ ada_w[l] + ada_b[l]
        mods = jnp.split(mod[:, None, :], N_MOD, axis=-1)
        shift_m, scale_m, gate_m, shift_f, scale_f, gate_f = mods

        h = rms_norm(x, mix_norm_g[l]) * (1.0 + scale_m) + shift_m
        proj = h @ w_in[l]
        p_pool, p_conv, p_sgu = jnp.split(proj, [POOL_WIDTH, POOL_WIDTH + 2 * CONV_WIDTH], axis=-1)
        out_a = multiscale_pool(p_pool, pool_w[l], pool_scale[l])
        out_b = conformer_conv(p_conv, conv_w[l], conv_b[l], conv_ln_g[l], conv_ln_b[l])
        out_c = spatial_gating(p_sgu, sgu_ln_g[l], sgu_ln_b[l], sgu_w[l], sgu_b[l])
        mixed = jnp.concatenate([out_a, out_b, out_c], axis=-1)
        x = x + gate_m * (mixed @ w_out[l])

        h = rms_norm(x, ffn_norm_g[l]) * (1.0 + scale_f) + shift_f
        i = l // 2
        if l % 2 == 0:
            y = swiglu(h, ffn_w_gate[i], ffn_w_up[i], ffn_w_down[i])
        else:
            y = moe_swiglu(h, router_w[i], router_b[i], moe_w_gate[i], moe_w_up[i], moe_w_down[i])
        x = x + gate_f * y
    return rms_norm(x, final_norm_g)
```

```python
import numpy as np
import concourse.bass as bass
import concourse.mybir as mybir
from concourse.bass_utils import run_bass_kernel_spmd

F32 = mybir.dt.float32
BF16 = mybir.dt.bfloat16
AF = mybir.ActivationFunctionType
ALU = mybir.AluOpType
AX = mybir.AxisListType

P = 128
TT = 512


class Buf:
    __slots__ = ("name", "w", "r", "dsem", "dcnt")

    def __init__(self, name):
        self.name = name
        self.w = {}
        self.r = {}
        self.dsem = None
        self.dcnt = 0


class Prog:
    ENGS = ("pe", "act", "dve", "pool", "sp")
    ROT = 20000

    def __init__(self, nc):
        self.nc = nc
        self.streams = {e: [] for e in self.ENGS}
        self.nsem = 0
        self.sem = {e: self._newsem(e) for e in self.ENGS}
        self.cnt = {e: 0 for e in self.ENGS}
        self.known = {e: {} for e in self.ENGS}
        self.final = []
        self.dma_evs = {}
        self.bg = set()

    def _newsem(self, tag):
        self.nsem += 1
        return self.nc.alloc_semaphore(f"s{self.nsem}_{tag}")

    def _collect(self, eng, reads, writes, pwrites=()):
        need = {}
        def add(ev):
            if ev is None:
                return
            s, v = ev
            k = id(s)
            if k not in need or need[k][1] < v:
                need[k] = (s, v)
        for b in reads:
            for ev in b.w.values():
                add(ev)
        for b in writes:
            for ev in b.w.values():
                add(ev)
            for ev in b.r.values():
                add(ev)
        for b in pwrites:
            for ev in b.r.values():
                add(ev)
        out = []
        kn = self.known[eng]
        for k, (s, v) in need.items():
            if kn.get(k, 0) >= v:
                continue
            kn[k] = v
            out.append((s, v))
        return out

    def op(self, eng, fn, reads=(), writes=()):
        waits = self._collect(eng, reads, writes)
        if self.cnt[eng] >= self.ROT:
            self.sem[eng] = self._newsem(eng)
            self.cnt[eng] = 0
        self.cnt[eng] += 1
        ev = (self.sem[eng], self.cnt[eng])
        self.streams[eng].append((fn, waits, (self.sem[eng], 1)))
        for b in reads:
            b.r[id(ev[0])] = ev
        for b in writes:
            b.w[id(ev[0])] = ev
            b.r = {}
        return ev

    def dma(self, q, fns, sb, reads=(), writes=(), pwrites=()):
        waits = self._collect(q, reads, writes, pwrites)
        if sb.dsem is None:
            sb.dsem = self._newsem("d_" + sb.name)
        for i, fn in enumerate(fns):
            self.streams[q].append((fn, waits if i == 0 else [], (sb.dsem, 16)))
        sb.dcnt += 16 * len(fns)
        ev = (sb.dsem, sb.dcnt)
        self.dma_evs[id(sb.dsem)] = ev
        for b in reads:
            b.r[id(ev[0])] = ev
        for b in writes:
            b.w[id(ev[0])] = ev
            b.r = {}
        for b in pwrites:
            b.w[id(ev[0])] = ev
        return ev

    def barrier(self):
        evs = [(self.sem[e], self.cnt[e]) for e in self.ENGS if self.cnt[e] > 0]
        evs += [ev for k, ev in self.dma_evs.items() if k not in self.bg]
        for e in self.ENGS:
            kn = self.known[e]
            waits = []
            for s_, v in evs:
                if kn.get(id(s_), 0) >= v:
                    continue
                kn[id(s_)] = v
                waits.append((s_, v))
            if waits:
                self.streams[e].append((None, waits, None))

    def emit(self):
        nc = self.nc
        engmap = {"pe": "tensor", "act": "scalar", "dve": "vector",
                  "pool": "gpsimd", "sp": "sync"}
        final = self.final
        with nc.Block() as block:
            for e in self.ENGS:
                stream = self.streams[e]
                last = (e == "sp")

                def body(eng, stream=stream, last=last):
                    for fn, waits, inc in stream:
                        for s, v in waits:
                            eng.wait_ge(s, v)
                        if fn is None:
                            continue
                        ins = fn(eng)
                        ins.then_inc(inc[0], inc[1])
                    if last:
                        fin = {}
                        for s, v in final:
                            if id(s) not in fin or fin[id(s)][1] < v:
                                fin[id(s)] = (s, v)
                        for s, v in fin.values():
                            eng.wait_ge(s, v)
                getattr(block, engmap[e])(body)


I32 = mybir.dt.int32
SB_LO = 16512
SB_WORDS = 53200
DIN = 1792
FB = 512
POOL_HALF = ((1, 2), (4, 8))


class Cfg:
    def __init__(self, S=8192, D=1024, n_layers=2, d_ff=2816, n_exp=8, d_ffe=3584):
        self.S = S
        self.D = D
        self.L = n_layers
        self.DFF = d_ff
        self.E = n_exp
        self.DFFE = d_ffe
        self.NT = S // TT
        self.DC = D // P
        self.sparse = True
        self.bgcast = True
        self.dense_ts = True


class T:
    def __init__(self, ap, name):
        self.ap = ap
        self.b = Buf(name)

    def __getitem__(self, k):
        return self.ap[k]


def _prod(xs):
    r = 1
    for v in xs:
        r *= v
    return r


class Arena:
    def __init__(self, big):
        self.big = big
        self.cur = 0
        self.n = 0

    def alloc(self, name, shape, dtype):
        esz = 2 if dtype == BF16 else 4
        n = _prod(shape[1:])
        nw = (n * esz + 3) // 4
        nw = (nw + 7) // 8 * 8
        w0 = self.cur
        self.cur += nw
        assert self.cur <= SB_WORDS, (name, self.cur * 4)
        ap = self.big[0:shape[0], w0:w0 + (n * esz + 3) // 4]
        if dtype != F32:
            ap = ap.bitcast(dtype)
        ap = ap[:, 0:n]
        if len(shape) == 3:
            ap = ap.rearrange("p (a b) -> p a b", a=shape[1])
        elif len(shape) == 4:
            ap = ap.rearrange("p (a b c) -> p a b c", a=shape[1], b=shape[2])
        self.n += 1
        return T(ap, f"{name}{self.n}")

    def mark(self):
        return self.cur

    def reset(self, m):
        self.cur = m


class Rot:
    def __init__(self, items):
        self.items = items
        self.i = 0

    def next(self):
        t = self.items[self.i % len(self.items)]
        self.i += 1
        return t


def build(cfg):
    nc = bass.Bass("TRN2", target_bir_lowering=False)
    S, D, DC, NT, L, E = cfg.S, cfg.D, cfg.DC, cfg.NT, cfg.L, cfg.E
    DFF, DFFE = cfg.DFF, cfg.DFFE
    pg = Prog(nc)

    def din(name, shape):
        return nc.dram_tensor(name, list(shape), F32, kind="ExternalInput").ap()

    nd = (L + 1) // 2
    nm = max(L // 2, 1)
    x_in = din("x", [S, D])
    c_pc = din("c_pc", [P, DC])
    ada_w = din("ada_w", [L, D, 6 * D])
    ada_b = din("ada_b_pc", [L, P, 6 * DC])
    mixg = din("mixg_pc", [L, P, DC])
    ffng = din("ffng_pc", [L, P, DC])
    w_in = din("w_in", [L, D, DIN])
    pool_w = din("pool_w", [L, 4, 64, 64])
    pool_scale = din("pool_scale", [L, 256])
    conv_wT = din("conv_wT", [L, 384, 31])
    conv_b = din("conv_b_pc", [L, P, 3])
    clng = din("conv_ln_g_pc", [L, P, 3])
    clnb = din("conv_ln_b_pc", [L, P, 3])
    slng = din("sgu_ln_g", [L, 384])
    slnb = din("sgu_ln_b", [L, 384])
    sgu_wT = din("sgu_wT", [L, 6, P, P])
    sgu_b = din("sgu_b", [L, 6 * P])
    w_out = din("w_out", [L, D, D])
    fwg = din("ffn_w_gate", [nd, D, DFF])
    fwu = din("ffn_w_up", [nd, D, DFF])
    fwd = din("ffn_w_down", [nd, DFF, D])
    rw = din("router_w", [nm, D, E])
    rb = din("router_b", [nm, E])
    mwg = din("moe_w_gate", [nm, E, D, DFFE])
    mwu = din("moe_w_up", [nm, E, D, DFFE])
    mwd = din("moe_w_down", [nm, E, DFFE, D])
    fng = din("final_norm_g", [D])
    ident_in = din("ident", [P, P])
    invcnt_in = din("invcnt", [2, 3, P, TT])
    sel_in = din("sel", [E, E * P])
    NTL = (2 * S) // TT + E
    NSLOT = NTL * TT
    NFBE = (DFFE + FB - 1) // FB
    NFCE = DFFE // P
    NBLK = S // P
    utri_in = din("utri", [P, P])
    rc_in = din("rc", [P, NTL + 1])
    H_tok = nc.dram_tensor("h_tok", [S, D], BF16).ap()
    H_slots = nc.dram_tensor("h_slots", [NSLOT, D], BF16).ap()
    Y_slots = nc.dram_tensor("y_slots", [NSLOT, D], F32).ap()
    WGU_r = nc.dram_tensor("wgu_r", [E * NFBE * P, 2 * DC * FB], BF16).ap()
    WD_r = nc.dram_tensor("wd_r", [E * P, NFCE * D], BF16).ap()
    wq_d = Buf("wq_d")
    FWg = nc.dram_tensor("fwg_bf", [nd, D, DFF], BF16).ap()
    FWu = nc.dram_tensor("fwu_bf", [nd, D, DFF], BF16).ap()
    FWd = nc.dram_tensor("fwd_bf", [nd, DFF, D], BF16).ap()
    fq_d = Buf("fq_d")
    htok_d = Buf("htok_d")
    hslots_d = Buf("hslots_d")
    yslots_d = [Buf(f"ysl{j}") for j in range(NTL)]
    moe_last = (L % 2 == 0)
    y_out = nc.dram_tensor("y", [S, D], F32, kind="ExternalOutput").ap()
    xT = nc.dram_tensor("xT_scr", [D, S], F32).ap()
    xT_v = xT.rearrange("(c p) t -> p c t", p=P)
    ocd = nc.dram_tensor("oc_scr", [384, S], BF16).ap()
    oc_v = ocd.rearrange("(c p) t -> p c t", p=P)
    xT_dram = [Buf(f"xTd{i}") for i in range(NT)]
    oc_dram = [Buf(f"ocd{i}") for i in range(NT)]

    big = nc.alloc_sbuf_tensor("big", [P, SB_WORDS], F32)
    ar = Arena(big)
    psum = [nc.alloc_psum_tensor(f"ps{i}", [P, 512], F32) for i in range(8)]
    b_ps = [Buf(f"ps{i}") for i in range(8)]

    def MM(out, pairs, reads, writes):
        pairs = list(pairs)

        def fn(e):
            ins = None
            n = len(pairs)
            for i, (l, r) in enumerate(pairs):
                ins = e.matmul(out, l, r, start=(i == 0), stop=(i == n - 1))
            return ins
        pg.op("pe", fn, reads, writes)

    def ACT(out, in_, func, reads, writes, bias=None, scale=None):
        kw = {}
        if bias is not None:
            kw["bias"] = bias
        if scale is not None:
            kw["scale"] = scale
        pg.op("act", lambda e: e.activation(out=out, in_=in_, func=func, **kw), reads, writes)

    def TTo(eng, out, in0, in1, op, reads, writes):
        pg.op(eng, lambda e: e.tensor_tensor(out=out, in0=in0, in1=in1, op=op), reads, writes)

    def TS(eng, out, in0, s1, op0, reads, writes, s2=0.0, op1=ALU.add):
        pg.op(eng, lambda e: e.tensor_scalar(out=out, in0=in0, scalar1=s1, scalar2=s2, op0=op0, op1=op1),
              reads, writes)

    def STT(eng, out, in0, scalar, in1, op0, op1, reads, writes):
        pg.op(eng, lambda e: e.scalar_tensor_tensor(out=out, in0=in0, scalar=scalar, in1=in1,
                                                    op0=op0, op1=op1), reads, writes)

    def CP(eng, out, in_, reads, writes):
        if eng == "act":
            pg.op("act", lambda e: e.copy(out=out, in_=in_), reads, writes)
        else:
            pg.op(eng, lambda e: e.tensor_copy(out=out, in_=in_), reads, writes)

    def MSET(eng, ap, val, writes):
        pg.op(eng, lambda e: e.memset(ap, val), (), writes)

    def RECIP(out, in_, reads, writes):
        pg.op("dve", lambda e: e.reciprocal(out=out, in_=in_), reads, writes)

    def LD(dst, dst_ap, src_ap, reads=(), q="sp"):
        return pg.dma(q, [lambda e: e.dma_start(out=dst_ap, in_=src_ap)], dst.b, reads=reads, writes=[dst.b])

    def ST(src, src_ap, dst_ap, dbuf, q="pool"):
        return pg.dma(q, [lambda e: e.dma_start(out=dst_ap, in_=src_ap)], src.b, reads=[src.b],
                      writes=([dbuf] if dbuf is not None else []))

    ident = ar.alloc("ident", [P, P], F32)
    ones_bf = ar.alloc("ones_bf", [P, P], BF16)
    ones_f = ar.alloc("ones_f", [P, P], F32)
    eps_t = ar.alloc("eps", [P, 8], F32)
    cond = ar.alloc("cond", [P, DC], F32)
    mod = [ar.alloc(f"mod{l}", [P, 6 * DC], F32) for l in range(L)]
    gscm = [ar.alloc(f"gscm{l}", [P, DC], F32) for l in range(L)]
    gscf = [ar.alloc(f"gscf{l}", [P, DC], F32) for l in range(L)]
    stage = Rot([ar.alloc(f"stg{i}", [P, 2048], F32) for i in range(2)])
    LD(ident, ident[:], ident_in)
    identb = ar.alloc("identb", [P, P], BF16)
    CP("dve", identb[:], ident[:], [ident.b], [identb.b])
    MSET("pool", ones_bf[:], 1.0, [ones_bf.b])
    MSET("pool", ones_f[:], 1.0, [ones_f.b])
    MSET("pool", eps_t[:], 1e-6, [eps_t.b])
    eps1 = eps_t[:, 0:1]

    def load_cast(dst, dst_ap, src_ap, fshape, eng="pool"):
        stg = stage.next()
        n = _prod(fshape)
        assert n <= 2048
        sv = stg[:, 0:n]
        if len(fshape) == 2:
            sv = sv.rearrange("p (a b) -> p a b", a=fshape[0])
        LD(stg, sv, src_ap)
        CP(eng, dst_ap, sv, [stg.b], [dst.b])

    slot1_i = ar.alloc("slot1_i", [P, NBLK], I32)
    slot2_i = ar.alloc("slot2_i", [P, NBLK], I32)
    w12 = ar.alloc("w12", [P, NBLK * 2], F32)
    pbase = ar.mark()

    wc_jobs = []

    def wc_build(li):
        for e_ in range(E):
            for fb in range(NFBE):
                f0 = fb * FB
                fw = min(FB, DFFE - f0)
                r0 = (e_ * NFBE + fb) * P
                rows = WGU_r[r0:r0 + P, :]
                cpp = max(1, 2048 // fw)
                for gu, src in ((0, mwg[li, e_]), (1, mwu[li, e_])):
                    sv = src[:, f0:f0 + fw].rearrange("(c p) f -> p c f", p=P)
                    for c0 in range(0, DC, cpp):
                        c1 = min(DC, c0 + cpp)
                        o0 = gu * DC * FB + c0 * FB
                        dst = rows[:, o0:o0 + (c1 - c0) * FB].rearrange("p (a b) -> p a b", a=c1 - c0)[:, :, 0:fw]
                        wc_jobs.append((sv[:, c0:c1, :], dst, c1 - c0, fw))
            dv = mwd[li, e_].rearrange("(j p) d -> p j d", p=P)
            rows = WD_r[e_ * P:(e_ + 1) * P, :].rearrange("p (j d) -> p j d", d=D)
            jpp = max(1, 2048 // D)
            for j0 in range(0, NFCE, jpp):
                j1 = min(NFCE, j0 + jpp)
                wc_jobs.append((dv[:, j0:j1, :], rows[:, j0:j1, :], j1 - j0, D))
    wc_state = {"eng": Rot(["act", "dve"])}

    def wc_run(k):
        for _ in range(k):
            if not wc_jobs:
                return
            src3, dst3, a, w = wc_jobs.pop(0)
            stg = wc_state["stg"].next()
            ob = wc_state["out"].next()
            n = a * w
            s3 = stg[:, 0:n].rearrange("p (a b) -> p a b", a=a)
            o3 = ob[:, 0:n].rearrange("p (a b) -> p a b", a=a)
            LD(stg, s3, src3)
            CP(wc_state["eng"].next(), o3, s3, [stg.b], [ob.b])
            pg.dma("pool", [lambda e, dst3=dst3, o3=o3: e.dma_start(out=dst3, in_=o3)], ob.b, reads=[ob.b],
                   pwrites=[wq_d])
    bgd_jobs = []
    bgd_buf = Buf("bgd")

    def bgd_build(li):
        for e_ in range(E):
            for fb in range(NFBE):
                f0 = fb * FB
                fw = min(FB, DFFE - f0)
                r0 = (e_ * NFBE + fb) * P
                rows = WGU_r[r0:r0 + P, :]
                for gu, src in ((0, mwg[li, e_]), (1, mwu[li, e_])):
                    sv = src[:, f0:f0 + fw].rearrange("(c p) f -> p c f", p=P)
                    o0 = gu * DC * FB
                    dst = rows[:, o0:o0 + DC * FB].rearrange("p (c f) -> p c f", c=DC)[:, :, 0:fw]
                    bgd_jobs.append((sv, dst))
            dv = mwd[li, e_].rearrange("(j p) d -> p j d", p=P)
            rows = WD_r[e_ * P:(e_ + 1) * P, :].rearrange("p (j d) -> p j d", d=D)
            for j0 in range(0, NFCE, 7):
                j1 = min(NFCE, j0 + 7)
                bgd_jobs.append((dv[:, j0:j1, :], rows[:, j0:j1, :]))

    def bgd_run(k):
        for _ in range(k):
            if not bgd_jobs:
                return
            src, dst = bgd_jobs.pop(0)
            pg.dma("pool", [lambda e, src=src, dst=dst: e.dma_start(out=dst, in_=src)], bgd_buf, pwrites=[wq_d])
            pg.bg.add(id(bgd_buf.dsem))
    if L >= 2 and cfg.sparse:
        if cfg.bgcast:
            bgd_build(0)
        else:
            wc_build(0)
            wc_state["stg"] = Rot(stage.items + [ar.alloc("stgw", [P, 2048], F32)])
            wc_state["out"] = Rot([ar.alloc(f"wcb{i}", [P, 2048], BF16) for i in range(3)])
    pwc = ar.mark()
    wc_total = len(wc_jobs)
    bgd_total = len(bgd_jobs)
    bgd_per = (bgd_total + 3 * NT - 1) // (3 * NT)

    adst = Rot([ar.alloc(f"adst{i}", [P, DC, D], F32) for i in range(2)])
    adab = ar.alloc("adab", [P, 6 * DC], F32)
    mg = ar.alloc("mg", [P, DC], F32)
    fg = ar.alloc("fg", [P, DC], F32)
    tmp8 = ar.alloc("tmp8", [P, DC], F32)
    modrow = ar.alloc("modrow", [1, 6 * D], F32)
    LD(cond, cond[:], c_pc)
    ACT(cond[:], cond[:], AF.Silu, [cond.b], [cond.b])
    for l in range(L):
        LD(adab, adab[:], ada_b[l])
        LD(mg, mg[:], mixg[l])
        LD(fg, fg[:], ffng[l])
        prot = Rot([2, 3])
        for m in range(6):
            st = adst.next()
            LD(st, st[:], ada_w[l][:, m * D:(m + 1) * D].rearrange("(c p) f -> p c f", p=P))
            for n0 in range(0, D, 512):
                k = prot.next()
                MM(psum[k][0:1, :], [(cond[:, c:c + 1], st[:, c, n0:n0 + 512]) for c in range(DC)],
                   [st.b, cond.b], [b_ps[k]])
                CP("act" if (n0 // 512) % 2 == 0 else "dve", modrow[0:1, m * D + n0:m * D + n0 + 512],
                   psum[k][0:1, :], [b_ps[k]], [modrow.b])
        for j in range(6 * DC):
            MM(psum[0][:, j:j + 1], [(modrow[0:1, j * P:(j + 1) * P], ones_f[0:1, 0:1])],
               [modrow.b, ones_f.b], [b_ps[0]])
        TTo("dve", mod[l][:], psum[0][:, 0:6 * DC], adab[:], ALU.add, [b_ps[0], adab.b], [mod[l].b])
        TS("dve", tmp8[:], mod[l][:, DC:2 * DC], 1.0, ALU.add, [mod[l].b], [tmp8.b])
        TTo("dve", gscm[l][:], tmp8[:], mg[:], ALU.mult, [tmp8.b, mg.b], [gscm[l].b])
        TS("dve", tmp8[:], mod[l][:, 4 * DC:5 * DC], 1.0, ALU.add, [mod[l].b], [tmp8.b])
        TTo("dve", gscf[l][:], tmp8[:], fg[:], ALU.mult, [tmp8.b, fg.b], [gscf[l].b])
    pg.barrier()
    ar.reset(pwc)


    dc_jobs = []
    for li_ in range(nd):
        for src, dst in ((fwg[li_], FWg[li_]), (fwu[li_], FWu[li_])):
            sv = src.rearrange("(c p) f -> p c f", p=P)
            dv_ = dst.rearrange("(c p) f -> p c f", p=P)
            for c in range(DC):
                for f0 in range(0, DFF, 2048):
                    f1 = min(DFF, f0 + 2048)
                    dc_jobs.append((sv[:, c, f0:f1], dv_[:, c, f0:f1], f1 - f0))
        sv = fwd[li_].rearrange("(j p) d -> p j d", p=P)
        dv_ = FWd[li_].rearrange("(j p) d -> p j d", p=P)
        for j0 in range(0, DFF // P, 2):
            j1 = min(DFF // P, j0 + 2)
            dc_jobs.append((sv[:, j0:j1, :], dv_[:, j0:j1, :], (j1 - j0, D)))
    dc_state = {"eng": Rot(["act", "dve"])}

    def dc_run(k):
        for _ in range(k):
            if not dc_jobs:
                return
            src, dst, shp = dc_jobs.pop(0)
            stg = dc_state["stg"].next()
            ob = dc_state["out"].next()
            if isinstance(shp, tuple):
                n = shp[0] * shp[1]
                s3 = stg[:, 0:n].rearrange("p (a b) -> p a b", a=shp[0])
                o3 = ob[:, 0:n].rearrange("p (a b) -> p a b", a=shp[0])
            else:
                s3 = stg[:, 0:shp]
                o3 = ob[:, 0:shp]
            LD(stg, s3, src)
            CP(dc_state["eng"].next(), o3, s3, [stg.b], [ob.b])
            pg.dma("pool", [lambda e, dst=dst, o3=o3: e.dma_start(out=dst, in_=o3)], ob.b, reads=[ob.b],
                   pwrites=[fq_d])

    dc_state["stg"] = Rot([ar.alloc(f"dcs{i}", [P, 2048], F32) for i in range(3)])
    dc_state["out"] = Rot([ar.alloc(f"dco{i}", [P, 2048], BF16) for i in range(3)])
    dc_per = (len(dc_jobs) + NT - 1) // NT
    wc_per0 = (len(wc_jobs) + NT - 1) // NT
    xin = Rot([ar.alloc(f"xin{i}", [P, 4, D], F32) for i in range(2)])
    xtr = Rot([ar.alloc(f"xt{i}", [P, DC, TT], F32) for i in range(2)])
    kk = 0
    for i in range(NT):
        xi = xin.next()
        xt = xtr.next()
        LD(xi, xi[:], x_in[i * TT:(i + 1) * TT, :].rearrange("(b p) d -> p b d", p=P))
        for c in range(DC):
            k = kk % 8
            kk += 1

            def tp(e, xi=xi, c=c, k=k):
                ins = None
                for b in range(4):
                    ins = e.transpose(psum[k][:, b * P:(b + 1) * P], xi[:, b, c * P:(c + 1) * P], ident[:])
                return ins
            pg.op("pe", tp, [xi.b, ident.b], [b_ps[k]])
            CP("act" if c % 2 == 0 else "dve", xt[:, c, :], psum[k][:], [b_ps[k]], [xt.b])
        ST(xt, xt[:], xT_v[:, :, i * TT:(i + 1) * TT], xT_dram[i])
        dc_run(dc_per)
        wc_run(wc_per0)
    dc_run(len(dc_jobs))
    wc_run(len(wc_jobs))
    pg.barrier()
    ar.reset(pbase)

    def norm_tile(xt, sq, rt, rstd, ssb, gsc, shift_ap, h_out, want_f32):
        hh = DC // 2
        for q in range(2):
            pg.op("act", lambda e, q=q: e.activation(out=sq[:, q * hh:(q + 1) * hh, :],
                                                     in_=xt[:, q * hh:(q + 1) * hh, :], func=AF.Square),
                  [xt.b], [sq.b])
        MM(psum[ssb][:], [(ones_bf[:], sq[:, c, :]) for c in range(DC)], [ones_bf.b, sq.b], [b_ps[ssb]])
        ACT(rt[:], psum[ssb][:], AF.Sqrt, [b_ps[ssb], eps_t.b], [rt.b], bias=eps1, scale=1.0 / D)
        RECIP(rstd[:], rt[:], [rt.b], [rstd.b])
        for c in range(DC):
            TTo("dve", xt[:, c, :], xt[:, c, :], rstd[:], ALU.mult, [xt.b, rstd.b], [xt.b])
            ACT(h_out(c), xt[:, c, :], AF.Identity, [xt.b, gsc.b], [h_out.T.b],
                bias=shift_ap(c), scale=gsc[:, c:c + 1])
            if want_f32:
                TS("dve", xt[:, c, :], xt[:, c, :], gsc[:, c:c + 1], ALU.mult, [xt.b, gsc.b], [xt.b],
                   s2=shift_ap(c), op1=ALU.add)

    class HOut:
        def __init__(self, T_, fn):
            self.T = T_
            self.fn = fn

        def __call__(self, c):
            return self.fn(c)

    def moe_sparse(l, li):
        md = mod[l]
        NBE = NBLK * E
        shift_f = lambda c: md[:, 3 * DC + c:3 * DC + c + 1]
        v3 = lambda t_: t_[:].rearrange("p (b e) -> p b e", e=E)
        eq1_all = ar.alloc("eq1_all", [P, NBE], F32)
        eq2_all = ar.alloc("eq2_all", [P, NBE], F32)
        mask_bf = ar.alloc("mask_bf", [P, NBE], BF16)
        utri_f = ar.alloc("utri_f", [P, P], F32)
        utri = ar.alloc("utri", [P, P], BF16)
        rc = ar.alloc("rc", [P, NTL + 1], F32)
        LD(utri_f, utri_f[:], utri_in)
        CP("dve", utri[:], utri_f[:], [utri_f.b], [utri.b])
        LD(rc, rc[:], rc_in)
        widx = ar.alloc("widx", [P, NTL * NFBE], I32)
        didx = ar.alloc("didx", [P, NTL], I32)
        m1base = ar.mark()
        rwt = ar.alloc("rwt", [P, DC, E], F32)
        LD(rwt, rwt[:], rw[li].rearrange("(c p) e -> p c e", p=P))
        rbb4 = ar.alloc("rbb4", [P, 4, E], F32)
        for blk in range(4):
            LD(rbb4, rbb4[:, blk, :], rb[li].partition_broadcast(P))
        xtr = Rot([ar.alloc(f"xt{i}", [P, DC, TT], F32) for i in range(2)])
        sq = ar.alloc("sq", [P, DC, TT], BF16)
        rt = ar.alloc("rt", [P, TT], F32)
        rstd = ar.alloc("rstd", [P, TT], F32)
        hbr = Rot([ar.alloc(f"hb{i}", [P, DC, TT], BF16) for i in range(2)])
        htr = Rot([ar.alloc(f"htok{i}", [P, 4, D], BF16) for i in range(2)])
        rsm = Rot([ar.alloc(f"rsm{i}", [P, 96], F32) for i in range(2)])
        trot = Rot([0, 1, 2, 3])
        def m1_norm(i):
            xt = xtr.next()
            hb_ = hbr.next()
            LD(xt, xt[:], xT_v[:, :, i * TT:(i + 1) * TT], reads=[xT_dram[i]])
            norm_tile(xt, sq, rt, rstd, 6, gscf[l], shift_f, HOut(hb_, lambda c, hb_=hb_: hb_[:, c, :]), True)
            return xt, hb_

        def m1_rest(i, xt, hb_):
            htok = htr.next()
            gb0 = i * 4
            r = rsm.next()
            lgt = r[:, 0:4 * E].rearrange("p (b e) -> p b e", e=E)
            lg2 = r[:, 4 * E:8 * E].rearrange("p (b e) -> p b e", e=E)
            m1 = r[:, 64:68]
            m2_ = r[:, 68:72]
            dd = r[:, 72:76]
            ex = r[:, 76:80]
            den = r[:, 80:84]
            eq1 = v3(eq1_all)[:, gb0:gb0 + 4, :]
            eq2 = v3(eq2_all)[:, gb0:gb0 + 4, :]
            w12v = w12[:].rearrange("p (b k) -> p b k", k=2)
            w1 = w12v[:, gb0:gb0 + 4, 0]
            w2 = w12v[:, gb0:gb0 + 4, 1]
            bc = lambda a_: a_.unsqueeze(2).to_broadcast([P, 4, E])
            for blk in range(4):
                MM(psum[7][:, blk * E:(blk + 1) * E],
                   [(xt[:, c, blk * P:(blk + 1) * P], rwt[:, c, :]) for c in range(DC)],
                   [xt.b, rwt.b], [b_ps[7]])
            TTo("dve", lgt, psum[7][:, 0:4 * E].rearrange("p (b e) -> p b e", e=E), rbb4[:], ALU.add,
                [b_ps[7], rbb4.b], [r.b])
            pg.op("dve", lambda e, m1=m1, lgt=lgt: e.tensor_reduce(out=m1, in_=lgt, axis=AX.X, op=ALU.max),
                  [r.b], [r.b])
            TTo("dve", eq1, lgt, bc(m1), ALU.is_equal, [r.b], [eq1_all.b])
            STT("dve", lg2, eq1, -1e30, lgt, ALU.mult, ALU.add, [r.b, eq1_all.b], [r.b])
            pg.op("dve", lambda e, m2_=m2_, lg2=lg2: e.tensor_reduce(out=m2_, in_=lg2, axis=AX.X, op=ALU.max),
                  [r.b], [r.b])
            TTo("dve", eq2, lg2, bc(m2_), ALU.is_equal, [r.b], [eq2_all.b])
            TTo("dve", dd, m2_, m1, ALU.subtract, [r.b], [r.b])
            ACT(ex, dd, AF.Exp, [r.b], [r.b])
            TS("dve", den, ex, 1.0, ALU.add, [r.b], [r.b])
            RECIP(w1, den, [r.b], [w12.b])
            TTo("dve", w2, ex, w1, ALU.mult, [r.b, w12.b], [w12.b])
            TTo("dve", v3(mask_bf)[:, gb0:gb0 + 4, :], eq1, eq2, ALU.add, [eq1_all.b, eq2_all.b], [mask_bf.b])
            for blk in range(4):
                k = trot.next()
                pbf = psum[k][:].bitcast(BF16)

                def tp(e, hb_=hb_, blk=blk, pbf=pbf):
                    ins = None
                    for c in range(DC):
                        ins = e.transpose(pbf[:, c * P:(c + 1) * P], hb_[:, c, blk * P:(blk + 1) * P], identb[:])
                    return ins
                pg.op("pe", tp, [hb_.b, identb.b], [b_ps[k]])
                CP("act" if blk % 2 == 0 else "dve", htok[:, blk, :], pbf[:, 0:D], [b_ps[k]], [htok.b])
            pg.dma("pool", [lambda e, htok=htok, i=i: e.dma_start(
                out=H_tok[i * TT:(i + 1) * TT, :].rearrange("(b p) d -> p b d", p=P), in_=htok[:])],
                htok.b, reads=[htok.b], pwrites=[htok_d])

        cur1 = m1_norm(0)
        for i in range(NT):
            nxt1 = m1_norm(i + 1) if i + 1 < NT else None
            m1_rest(i, cur1[0], cur1[1])
            cur1 = nxt1
        pg.barrier()
        ar.reset(m1base)
        sa = ar.alloc("sa", [P, NBE], F32)
        sb_ = ar.alloc("sb", [P, NBE], F32)
        tot = ar.alloc("tot", [P, NBE], F32)
        slot_all = ar.alloc("slot_all", [P, NBE], F32)
        prod = ar.alloc("prod", [P, NBE], F32)
        sm = ar.alloc("sm", [P, 16 * E], F32)
        ejt = ar.alloc("ejt", [P, 4 * NTL], F32)
        sf = ar.alloc("sf", [P, 2 * NBLK], F32)
        MM(psum[0][:, 0:NBE], [(utri[:], mask_bf[:])], [utri.b, mask_bf.b], [b_ps[0]])
        MM(psum[1][:, 0:NBE], [(ones_bf[:], mask_bf[:])], [ones_bf.b, mask_bf.b], [b_ps[1]])
        CP("dve", tot[:], psum[1][:, 0:NBE], [b_ps[1]], [tot.b])
        CP("dve", sa[:], tot[:], [tot.b], [sa.b])
        cur, nxt = sa, sb_
        sh = 1
        while sh < NBLK:
            CP("dve", v3(nxt)[:, 0:sh, :], v3(cur)[:, 0:sh, :], [cur.b], [nxt.b])
            TTo("dve", v3(nxt)[:, sh:NBLK, :], v3(cur)[:, sh:NBLK, :], v3(cur)[:, 0:NBLK - sh, :], ALU.add,
                [cur.b], [nxt.b])
            cur, nxt = nxt, cur
            sh *= 2
        incl = cur
        boff = nxt
        TTo("dve", boff[:], incl[:], tot[:], ALU.subtract, [incl.b, tot.b], [boff.b])
        n_e = v3(incl)[:, NBLK - 1, :]
        pa = sm[:, 0:E]
        pm = sm[:, E:2 * E]
        pad = sm[:, 2 * E:3 * E]
        st_ = sm[:, 3 * E:4 * E]
        en_ = sm[:, 4 * E:5 * E]
        for e_ in range(E):
            TS("dve", ejt[:, 0:NTL], rc[:, 0:NTL], v3(incl)[:, NBLK - 1, e_:e_ + 1], ALU.is_lt,
               [rc.b, incl.b, ejt.b], [ejt.b])
            pg.op("dve", lambda e, e_=e_: e.tensor_reduce(out=pa[:, e_:e_ + 1], in_=ejt[:, 0:NTL], axis=AX.X, op=ALU.add),
                  [ejt.b], [sm.b])
        TS("dve", pad, pa, float(TT), ALU.mult, [sm.b], [sm.b])
        MSET("dve", st_[:, 0:1], 0.0, [sm.b])
        for e_ in range(1, E):
            TTo("dve", st_[:, e_:e_ + 1], st_[:, e_ - 1:e_], pad[:, e_ - 1:e_], ALU.add, [sm.b], [sm.b])
        TTo("dve", en_, st_, pad, ALU.add, [sm.b], [sm.b])
        for e_ in range(E):
            TS("dve", v3(boff)[:, :, e_], v3(boff)[:, :, e_], st_[:, e_:e_ + 1], ALU.add, [boff.b, sm.b], [boff.b])
        TTo("dve", slot_all[:], psum[0][:, 0:NBE], boff[:], ALU.add, [b_ps[0], boff.b], [slot_all.b])
        for eq_, sl_i, o in ((eq1_all, slot1_i, 0), (eq2_all, slot2_i, NBLK)):
            TTo("dve", prod[:], eq_[:], slot_all[:], ALU.mult, [eq_.b, slot_all.b], [prod.b])
            pg.op("dve", lambda e, o=o: e.tensor_reduce(out=sf[:, o:o + NBLK], in_=v3(prod), axis=AX.X, op=ALU.add),
                  [prod.b], [sf.b])
            CP("dve", sl_i[:], sf[:, o:o + NBLK], [sf.b], [sl_i.b])
        jv = rc[:, 0:NTL]
        iop = rc[:, NTL:NTL + 1]
        ej = ejt[:, 0:NTL]
        tmpj = ejt[:, NTL:2 * NTL]
        basef = ejt[:, 2 * NTL:3 * NTL]
        tmpk = ejt[:, 3 * NTL:4 * NTL]
        MSET("dve", ej, 0.0, [ejt.b])
        for e_ in range(E):
            TS("dve", tmpj, jv, en_[:, e_:e_ + 1], ALU.is_ge, [rc.b, sm.b, ejt.b], [ejt.b])
            TTo("dve", ej, ej, tmpj, ALU.add, [ejt.b], [ejt.b])
        TS("dve", ej, ej, float(E - 1), ALU.min, [ejt.b], [ejt.b])
        TS("dve", basef, ej, float(NFBE * P), ALU.mult, [ejt.b], [ejt.b])
        wv = widx[:].rearrange("p (j f) -> p j f", f=NFBE)
        for fb in range(NFBE):
            TS("dve", tmpk, basef, iop, ALU.add, [ejt.b, rc.b], [ejt.b], s2=float(fb * P), op1=ALU.add)
            CP("dve", wv[:, :, fb], tmpk, [ejt.b], [widx.b])
        TS("dve", tmpk, ej, float(P), ALU.mult, [ejt.b, rc.b], [ejt.b], s2=iop, op1=ALU.add)
        CP("dve", didx[:], tmpk, [ejt.b], [didx.b])
        h2r = Rot([ar.alloc(f"h2{i}", [P, D], BF16) for i in range(4)])
        for gb in range(NBLK):
            h2 = h2r.next()
            LD(h2, h2[:], H_tok[gb * P:(gb + 1) * P, :], reads=[htok_d])
            for sl_i in (slot1_i, slot2_i):
                pg.dma("pool", [lambda e, h2=h2, sl_i=sl_i, gb=gb: e.indirect_dma_start(
                    out=H_slots[:, :], out_offset=bass.IndirectOffsetOnAxis(ap=sl_i[:, gb:gb + 1], axis=0),
                    in_=h2[:], in_offset=None, bounds_check=None)],
                    h2.b, reads=[h2.b, sl_i.b], pwrites=[hslots_d])
        pg.barrier()
        ar.reset(m1base)
        hsr = Rot([ar.alloc(f"hs{i}", [P, 4, D], BF16) for i in range(2)])
        hgr = Rot([ar.alloc(f"hg{i}", [P, DC, TT], BF16) for i in range(2)])
        aT = ar.alloc("aT", [P, NFCE, TT], BF16)
        wd_ = ar.alloc("wdx", [P, NFCE, D], BF16)
        wgur = Rot([ar.alloc(f"wgu{i}", [P, 2, DC, FB], BF16) for i in range(2)])
        sgr = Rot([ar.alloc(f"sg{i}", [P, TT], BF16) for i in range(2)])
        ytr = Rot([ar.alloc(f"yt{i}", [P, D], F32) for i in range(2)])
        grot = Rot([0, 1])
        urot = Rot([2, 3])
        yrot = Rot([4, 5])
        t2rot = Rot([6, 7])
        def t_load(j):
            hs = hsr.next()
            LD(hs, hs[:], H_slots[j * TT:(j + 1) * TT, :].rearrange("(b p) d -> p b d", p=P), reads=[hslots_d])
            return hs

        def t_transpose(j, hs):
            hg = hgr.next()
            for c in range(DC):
                k = t2rot.next()
                pbf = psum[k][:].bitcast(BF16)

                def tp(e, hs=hs, c=c, pbf=pbf):
                    ins = None
                    for b_ in range(4):
                        ins = e.transpose(pbf[:, b_ * P:(b_ + 1) * P], hs[:, b_, c * P:(c + 1) * P], identb[:])
                    return ins
                pg.op("pe", tp, [hs.b, identb.b], [b_ps[k]])
                CP("act" if c % 2 == 0 else "dve", hg[:, c, :], pbf[:, 0:TT], [b_ps[k]], [hg.b])
            return hg

        def t_wd(j):
            pg.dma("pool", [lambda e, j=j: e.indirect_dma_start(
                out=wd_[:].rearrange("p j d -> p (j d)"), out_offset=None, in_=WD_r[:, :],
                in_offset=bass.IndirectOffsetOnAxis(ap=didx[:, j:j + 1], axis=0),
                bounds_check=None)],
                wd_.b, reads=[didx.b, wq_d], writes=[wd_.b])

        wgu_of = {}

        def t_gather(j, fb):
            if j >= NTL:
                return
            wgu = wgur.next()
            wgu_of[(j, fb)] = wgu
            pg.dma("pool", [lambda e, j=j, fb=fb, wgu=wgu: e.indirect_dma_start(
                out=wgu[:].rearrange("p a c f -> p (a c f)"), out_offset=None, in_=WGU_r[:, :],
                in_offset=bass.IndirectOffsetOnAxis(ap=widx[:, j * NFBE + fb:j * NFBE + fb + 1], axis=0),
                bounds_check=None)],
                wgu.b, reads=[widx.b, wq_d], writes=[wgu.b])

        NAH = len(wgur.items)
        order = [(j, fb) for j in range(NTL) for fb in range(NFBE)]
        hs_cur = t_load(0)
        hg_cur = t_transpose(0, hs_cur)
        for q_ in range(min(NAH, len(order))):
            t_gather(*order[q_])
        t_wd(0)
        gi = NAH
        for j in range(NTL):
            hg = hg_cur
            hs_next = t_load(j + 1) if j + 1 < NTL else None
            for fb in range(NFBE):
                f0 = fb * FB
                fw = min(FB, DFFE - f0)
                wgu = wgu_of.pop((j, fb))
                for fc in range(fw // P):
                    kg = grot.next()
                    ku = urot.next()
                    MM(psum[kg][:], [(wgu[:, 0, c, fc * P:(fc + 1) * P], hg[:, c, :]) for c in range(DC)],
                       [wgu.b, hg.b], [b_ps[kg]])
                    MM(psum[ku][:], [(wgu[:, 1, c, fc * P:(fc + 1) * P], hg[:, c, :]) for c in range(DC)],
                       [wgu.b, hg.b], [b_ps[ku]])
                    sg = sgr.next()
                    ACT(sg[:], psum[kg][:], AF.Silu, [b_ps[kg]], [sg.b])
                    TTo("dve", aT[:, f0 // P + fc, :], psum[ku][:], sg[:], ALU.mult, [b_ps[ku], sg.b], [aT.b])
                if gi < len(order):
                    t_gather(*order[gi])
                    gi += 1
            if hs_next is not None:
                hg_cur = t_transpose(j + 1, hs_next)
            for b_ in range(4):
                yt = ytr.next()
                for hf_ in range(2):
                    ky = yrot.next()
                    MM(psum[ky][:], [(aT[:, f, b_ * P:(b_ + 1) * P], wd_[:, f, hf_ * 512:(hf_ + 1) * 512])
                                     for f in range(NFCE)], [aT.b, wd_.b], [b_ps[ky]])
                    CP("act" if hf_ == 0 else "dve", yt[:, hf_ * 512:(hf_ + 1) * 512], psum[ky][:], [b_ps[ky]], [yt.b])
                r0 = j * TT + b_ * P
                pg.dma("sp", [lambda e, yt=yt, r0=r0: e.dma_start(out=Y_slots[r0:r0 + P, :], in_=yt[:])],
                       yt.b, reads=[yt.b], pwrites=[yslots_d[j]])
            if j + 1 < NTL:
                t_wd(j + 1)

    def ffn_dense_ts(l, li):
        md = mod[l]
        NFBD = (DFF + FB - 1) // FB
        NFCD = DFF // P
        gate_f = lambda dc: md[:, 5 * DC + dc:5 * DC + dc + 1]
        shift_f = lambda c: md[:, 3 * DC + c:3 * DC + c + 1]
        wdf = ar.alloc("wdf", [P, NFCD, D], BF16)
        dvv = FWd[li].rearrange("(j p) d -> p j d", p=P)
        for j0 in range(0, NFCD, 6):
            j1 = min(NFCD, j0 + 6)
            LD(wdf, wdf[:, j0:j1, :], dvv[:, j0:j1, :], reads=[fq_d])
        aTd = ar.alloc("aTd", [P, NFCD, TT], BF16)
        wgur = Rot([ar.alloc(f"wgd{i}", [P, 2, DC, FB], BF16) for i in range(3)])
        xt1 = ar.alloc("xtn", [P, DC, TT], F32)
        xr1 = ar.alloc("xr", [P, DC, TT], F32)
        sq = ar.alloc("sq", [P, DC, TT], BF16)
        rt = ar.alloc("rt", [P, TT], F32)
        rstd = ar.alloc("rstd", [P, TT], F32)
        hbr = Rot([ar.alloc(f"hd{i}", [P, DC, TT], BF16) for i in range(2)])
        sgr = Rot([ar.alloc(f"sgd{i}", [P, TT], BF16) for i in range(2)])
        grot = Rot([0, 1])
        urot = Rot([2, 3])
        yrot = Rot([4, 5])
        order = [(i, fb) for i in range(NT) for fb in range(NFBD)]
        wof = {}

        def wload(i, fb):
            w_ = wgur.next()
            wof[(i, fb)] = w_
            f0 = fb * FB
            fw = min(FB, DFF - f0)
            LD(w_, w_[:, 0, :, 0:fw], FWg[li][:, f0:f0 + fw].rearrange("(c p) f -> p c f", p=P), reads=[fq_d])
            LD(w_, w_[:, 1, :, 0:fw], FWu[li][:, f0:f0 + fw].rearrange("(c p) f -> p c f", p=P), reads=[fq_d])

        def dnorm(i):
            h = hbr.next()
            LD(xt1, xt1[:], xT_v[:, :, i * TT:(i + 1) * TT], reads=[xT_dram[i]])
            norm_tile(xt1, sq, rt, rstd, 6, gscf[l], shift_f, HOut(h, lambda c, h=h: h[:, c, :]), False)
            return h
        NAH = len(wgur.items)
        for q_ in range(min(NAH, len(order))):
            wload(*order[q_])
        gi = NAH
        hcur = dnorm(0)
        for i in range(NT):
            if l == 0:
                bgd_run(bgd_per)
            h = hcur
            LD(xr1, xr1[:], xT_v[:, :, i * TT:(i + 1) * TT], reads=[xT_dram[i]])
            for fb in range(NFBD):
                f0 = fb * FB
                fw = min(FB, DFF - f0)
                w_ = wof.pop((i, fb))
                for fc in range(fw // P):
                    kg = grot.next()
                    ku = urot.next()
                    MM(psum[kg][:], [(w_[:, 0, c, fc * P:(fc + 1) * P], h[:, c, :]) for c in range(DC)],
                       [w_.b, h.b], [b_ps[kg]])
                    MM(psum[ku][:], [(w_[:, 1, c, fc * P:(fc + 1) * P], h[:, c, :]) for c in range(DC)],
                       [w_.b, h.b], [b_ps[ku]])
                    sg = sgr.next()
                    ACT(sg[:], psum[kg][:], AF.Silu, [b_ps[kg]], [sg.b])
                    TTo("dve", aTd[:, f0 // P + fc, :], psum[ku][:], sg[:], ALU.mult, [b_ps[ku], sg.b], [aTd.b])
                if gi < len(order):
                    wload(*order[gi])
                    gi += 1
            for dc in range(DC):
                ky = yrot.next()
                MM(psum[ky][:], [(wdf[:, f, dc * P:(dc + 1) * P], aTd[:, f, :]) for f in range(NFCD)],
                   [wdf.b, aTd.b], [b_ps[ky]])
                STT("dve", xr1[:, dc, :], psum[ky][:], gate_f(dc), xr1[:, dc, :], ALU.mult, ALU.add,
                    [b_ps[ky], md.b, xr1.b], [xr1.b])
            ST(xr1, xr1[:], xT_v[:, :, i * TT:(i + 1) * TT], xT_dram[i])
            hcur = dnorm(i + 1) if i + 1 < NT else None
        bgd_run(len(bgd_jobs))

    for l in range(L):
        md = mod[l]
        a_all = ar.alloc("a_all", [P, 2, S + 32], BF16)
        glu_all = ar.alloc("glu_all", [P, 3, S + 32], BF16)
        for tl in (a_all, glu_all):
            MSET("pool", tl[:, :, 0:16], 0.0, [tl.b])
            MSET("pool", tl[:, :, S + 16:S + 32], 0.0, [tl.b])
        p1base = ar.mark()
        win = ar.alloc("win", [P, DC, DIN], BF16)
        for c in range(DC):
            load_cast(win, win[:, c, :], w_in[l][c * P:(c + 1) * P, :], (DIN,))
        lngb = ar.alloc("lngb", [P, 384], F32)
        lnbb = ar.alloc("lnbb", [P, 384], F32)
        LD(lngb, lngb[:], slng[l].partition_broadcast(P))
        LD(lnbb, lnbb[:], slnb[l].partition_broadcast(P))
        swT = ar.alloc("swT", [P, 6, P], BF16)
        load_cast(swT, swT[:], sgu_wT[l].rearrange("h q p -> q h p"), (6, P))
        biasT = ar.alloc("biasT", [P, 3, P], F32)
        for j in range(3):
            for hh in range(2):
                hd = 2 * j + hh
                LD(biasT, biasT[hh * 64:(hh + 1) * 64, j, :], sgu_b[l, hd * P:(hd + 1) * P].partition_broadcast(64))
        xtr = Rot([ar.alloc(f"xt{i}", [P, DC, TT], F32) for i in range(1)])
        sq = ar.alloc("sq", [P, DC, TT], BF16)
        hb = Rot([ar.alloc(f"h{i}", [P, DC, TT], BF16) for i in range(2)])
        rt = ar.alloc("rt", [P, TT], F32)
        rstd = ar.alloc("rstd", [P, TT], F32)
        sgr = Rot([ar.alloc(f"sg{i}", [P, TT], F32) for i in range(2)])
        st6 = Rot([ar.alloc(f"st6{i}", [P, 8], F32) for i in range(2)])
        mv = Rot([ar.alloc(f"mv{i}", [P, 8], F32) for i in range(2)])
        vt = Rot([ar.alloc(f"vt{i}", [P, 384], F32) for i in range(2)])
        vn = Rot([ar.alloc(f"vn{i}", [P, 384], BF16) for i in range(3)])
        mxs = Rot([ar.alloc(f"mxs{i}", [P, TT], F32) for i in range(1)])
        octr = Rot([ar.alloc(f"oct{i}", [P, 3, TT], BF16) for i in range(2)])
        pj = Rot([1, 2])
        vpr = Rot([3, 4])
        def p1a_normA(i):
            xt = xtr.next()
            h = hb.next()
            LD(xt, xt[:], xT_v[:, :, i * TT:(i + 1) * TT], reads=[xT_dram[i]])
            hh_ = DC // 2
            for q in range(2):
                pg.op("act", lambda e, q=q, xt=xt: e.activation(out=sq[:, q * hh_:(q + 1) * hh_, :],
                                                               in_=xt[:, q * hh_:(q + 1) * hh_, :], func=AF.Square),
                      [xt.b], [sq.b])
            MM(psum[0][:], [(ones_bf[:], sq[:, c, :]) for c in range(DC)], [ones_bf.b, sq.b], [b_ps[0]])
            ACT(rt[:], psum[0][:], AF.Sqrt, [b_ps[0], eps_t.b], [rt.b], bias=eps1, scale=1.0 / D)
            RECIP(rstd[:], rt[:], [rt.b], [rstd.b])
            return (xt, h)

        def p1a_normB(st, c):
            xt, h = st
            TTo("dve", xt[:, c, :], xt[:, c, :], rstd[:], ALU.mult, [xt.b, rstd.b], [xt.b])
            ACT(h[:, c, :], xt[:, c, :], AF.Identity, [xt.b, gscm[l].b, md.b], [h.b],
                bias=md[:, c:c + 1], scale=gscm[l][:, c:c + 1])

        def p1a_front(i, h, nxt):
            base = 16 + i * TT

            def proj(f, k):
                MM(psum[k][:], [(win[:, c, f * P:(f + 1) * P], h[:, c, :]) for c in range(DC)],
                   [win.b, h.b], [b_ps[k]])
            for f in range(2):
                k = pj.next()
                proj(f, k)
                CP("act", a_all[:, f, base:base + TT], psum[k][:], [b_ps[k]], [a_all.b])
                if nxt is not None:
                    p1a_normB(nxt, f)
            for j in range(3):
                k = pj.next()
                proj(5 + j, k)
                sg = sgr.next()
                ACT(sg[:], psum[k][:], AF.Sigmoid, [b_ps[k]], [sg.b])
                if nxt is not None:
                    p1a_normB(nxt, 2 + 2 * j)
                k = pj.next()
                proj(2 + j, k)
                TTo("dve", glu_all[:, j, base:base + TT], psum[k][:], sg[:], ALU.mult,
                    [b_ps[k], sg.b], [glu_all.b])
                if nxt is not None:
                    p1a_normB(nxt, 3 + 2 * j)

        def p1a_sgu(i, h):
            base = 16 + i * TT

            def proj(f, k):
                MM(psum[k][:], [(win[:, c, f * P:(f + 1) * P], h[:, c, :]) for c in range(DC)],
                   [win.b, h.b], [b_ps[k]])
            def sgu_chain(blk):
                k = vpr.next()
                MM(psum[k][:, 0:384],
                   [(h[:, c, blk * P:(blk + 1) * P], win[:, c, 1408:1792]) for c in range(DC)],
                   [win.b, h.b], [b_ps[k]])
                s6 = st6.next()
                m2 = mv.next()
                v1 = vt.next()
                v2 = vn.next()
                pg.op("dve", lambda e, s6=s6, k=k: e.bn_stats(out=s6[:, 0:6], in_=psum[k][:, 0:384]),
                      [b_ps[k]], [s6.b])
                pg.op("dve", lambda e, s6=s6, m2=m2: e.bn_aggr(out=m2[:, 0:2], in_=s6[:, 0:6]),
                      [s6.b], [m2.b])
                ACT(m2[:, 2:3], m2[:, 1:2], AF.Sqrt, [m2.b, eps_t.b], [m2.b], bias=eps1, scale=1.0)
                RECIP(m2[:, 3:4], m2[:, 2:3], [m2.b], [m2.b])
                TS("dve", v1[:], psum[k][:, 0:384], m2[:, 0:1], ALU.subtract, [b_ps[k], m2.b], [v1.b],
                   s2=m2[:, 3:4], op1=ALU.mult)
                TTo("pool", v1[:], v1[:], lngb[:], ALU.mult, [v1.b, lngb.b], [v1.b])
                TTo("pool", v2[:], v1[:], lnbb[:], ALU.add, [v1.b, lnbb.b], [v2.b])
                return v2

            def sgu_spatial(blk, v2):
                for j in range(3):
                    def sp(e, j=j, blk=blk, v2=v2):
                        ins = None
                        for hh in range(2):
                            hd = 2 * j + hh
                            o = psum[5 + j][hh * 64:(hh + 1) * 64, blk * P:(blk + 1) * P]
                            ins = e.matmul(o, v2[:, hd * 64:(hd + 1) * 64], swT[:, hd, :], start=True, stop=True)
                        return ins
                    pg.op("pe", sp, [v2.b, swT.b], [b_ps[5 + j]])

            v2s = {}
            v2s[0] = sgu_chain(0)
            v2s[1] = sgu_chain(1)
            sgu_spatial(0, v2s[0])
            v2s[2] = sgu_chain(2)
            sgu_spatial(1, v2s[1])
            v2s[3] = sgu_chain(3)
            sgu_spatial(2, v2s[2])
            sgu_spatial(3, v2s[3])
            oct_ = octr.next()
            for j in range(3):
                mx = mxs.next()
                TTo("dve", mx[:].rearrange("p (b q) -> p b q", b=4), psum[5 + j][:].rearrange("p (b q) -> p b q", b=4),
                    biasT[:, j, :].unsqueeze(1).to_broadcast([P, 4, P]), ALU.add, [b_ps[5 + j], biasT.b], [mx.b])
                k = pj.next()
                proj(8 + j, k)
                TTo("dve", oct_[:, j, :], psum[k][:], mx[:], ALU.mult, [b_ps[k], mx.b], [oct_.b])
            ST(oct_, oct_[:], oc_v[:, :, i * TT:(i + 1) * TT], oc_dram[i])

        st0 = p1a_normA(0)
        for c in range(DC):
            p1a_normB(st0, c)
        hcur = st0[1]
        for i in range(NT):
            if l == 0:
                bgd_run(bgd_per)
            nxt = p1a_normA(i + 1) if i + 1 < NT else None
            p1a_front(i, hcur, nxt)
            p1a_sgu(i, hcur)
            hcur = nxt[1] if nxt is not None else None
        pg.barrier()
        ar.reset(p1base)

        wout = ar.alloc("wout", [P, DC, D], BF16)
        for c in range(DC):
            load_cast(wout, wout[:, c, :], w_out[l][c * P:(c + 1) * P, :], (D,))
        wbf = ar.alloc("wbf", [P, 2, P], F32)
        psb = ar.alloc("psb", [P, 2 * P], F32)
        wbd = ar.alloc("wbd", [P, 2, P], BF16)
        wbh = ar.alloc("wbh", [P, 2, P], BF16)
        MSET("pool", wbf[:], 0.0, [wbf.b])
        MSET("pool", wbh[:], 0.0, [wbh.b])
        for c in range(2):
            for g in range(2):
                LD(wbf, wbf[g * 64:(g + 1) * 64, c, g * 64:(g + 1) * 64], pool_w[l, 2 * c + g])
        LD(psb, psb[:], pool_scale[l].partition_broadcast(P))
        TTo("dve", wbd[:], wbf[:], psb[:].rearrange("p (c f) -> p c f", c=2), ALU.mult, [wbf.b, psb.b], [wbd.b])
        CP("dve", wbh[64:128, :, :], wbd[64:128, :, :], [wbd.b], [wbh.b])
        cwT = ar.alloc("cwT", [P, 3, 31], F32)
        LD(cwT, cwT[:], conv_wT[l].rearrange("(c p) k -> p c k", p=P))
        dg = ar.alloc("dg", [P, 93, P], BF16)
        for c in range(3):
            for k in range(31):
                TS("pool" if (k % 2) else "dve", dg[:, c * 31 + k, :], ident[:], cwT[:, c, k:k + 1], ALU.mult,
                   [ident.b, cwT.b], [dg.b])
        cb = ar.alloc("cb", [P, 3], F32)
        lg_ = ar.alloc("lg", [P, 3], F32)
        lb_ = ar.alloc("lb", [P, 3], F32)
        LD(cb, cb[:], conv_b[l])
        LD(lg_, lg_[:], clng[l])
        LD(lb_, lb_[:], clnb[l])
        invc = ar.alloc("invc", [P, 2, 3, TT], F32)
        LD(invc, invc[:], invcnt_in.rearrange("c v p t -> p c v t"))
        xtr = Rot([ar.alloc(f"xt{i}", [P, DC, TT], F32) for i in range(1)])
        mtr = Rot([ar.alloc(f"mt{i}", [P, 5, TT], BF16) for i in range(2)])
        octr = Rot([ar.alloc(f"oct{i}", [P, 3, TT], BF16) for i in range(2)])
        yb = ar.alloc("yb", [P, 3, TT], BF16)
        ysq = ar.alloc("ysq", [P, 3, TT], BF16)
        t1 = Rot([ar.alloc(f"t1{i}", [P, TT], F32) for i in range(1)])
        mm_ = ar.alloc("m", [P, TT], F32)
        msq = ar.alloc("msq", [P, TT], F32)
        var_ = ar.alloc("var", [P, TT], F32)
        crs = ar.alloc("crs", [P, TT], F32)
        tcr = Rot([ar.alloc(f"tc{i}", [P, TT], F32) for i in range(1)])
        yr = Rot([2, 3])
        orot = Rot([6, 7])
        def p1b_A(i):
            base = 16 + i * TT
            vr = 0 if i == 0 else (2 if i == NT - 1 else 1)
            mt = mtr.next()
            oct_ = octr.next()
            LD(oct_, oct_[:], oc_v[:, :, i * TT:(i + 1) * TT], reads=[oc_dram[i]])
            for c in range(2):
                h0, h1 = POOL_HALF[c]
                pairs = []
                for j in range(-h1, h1):
                    w_ = wbd if (-h0 <= j < h0) else wbh
                    pairs.append((w_[:, c, :], a_all[:, c, base + j:base + j + TT]))
                MM(psum[0][:], pairs, [wbd.b, wbh.b, a_all.b], [b_ps[0]])
                MM(psum[1][:], [(wbd[:, c, :], a_all[:, c, base:base + TT])], [wbd.b, a_all.b], [b_ps[1]])
                tt1 = t1.next()
                TTo("dve", tt1[:], psum[0][:], invc[:, c, vr, :], ALU.mult, [b_ps[0], invc.b], [tt1.b])
                TTo("dve", mt[:, c, :], tt1[:], psum[1][:], ALU.subtract, [tt1.b, b_ps[1]], [mt.b])
            for c in range(3):
                k = yr.next()
                MM(psum[k][:], [(dg[:, c * 31 + kk_, :], glu_all[:, c, base + kk_ - 15:base + kk_ - 15 + TT])
                                for kk_ in range(31)], [dg.b, glu_all.b], [b_ps[k]])
                ACT(yb[:, c, :], psum[k][:], AF.Identity, [b_ps[k], cb.b], [yb.b], bias=cb[:, c:c + 1], scale=1.0)
                ACT(ysq[:, c, :], psum[k][:], AF.Square, [b_ps[k], cb.b], [ysq.b], bias=cb[:, c:c + 1], scale=1.0)
            MM(psum[4][:], [(ones_bf[:], yb[:, c, :]) for c in range(3)], [ones_bf.b, yb.b], [b_ps[4]])
            MM(psum[5][:], [(ones_bf[:], ysq[:, c, :]) for c in range(3)], [ones_bf.b, ysq.b], [b_ps[5]])
            ACT(mm_[:], psum[4][:], AF.Identity, [b_ps[4]], [mm_.b], scale=1.0 / 384.0)
            TTo("dve", msq[:], mm_[:], mm_[:], ALU.mult, [mm_.b], [msq.b])
            STT("dve", var_[:], psum[5][:], 1.0 / 384.0, msq[:], ALU.mult, ALU.subtract,
                [b_ps[5], msq.b], [var_.b])
            ACT(var_[:], var_[:], AF.Sqrt, [var_.b, eps_t.b], [var_.b], bias=eps1, scale=1.0)
            RECIP(crs[:], var_[:], [var_.b], [crs.b])
            for c in range(3):
                tc_ = tcr.next()
                TTo("dve", tc_[:], yb[:, c, :], mm_[:], ALU.subtract, [yb.b, mm_.b], [tc_.b])
                TTo("dve", tc_[:], tc_[:], crs[:], ALU.mult, [tc_.b, crs.b], [tc_.b])
                ACT(mt[:, 2 + c, :], tc_[:], AF.Silu, [tc_.b, lg_.b, lb_.b], [mt.b],
                    bias=lb_[:, c:c + 1], scale=lg_[:, c:c + 1])
            return mt, oct_

        def p1b_B(i, mt, oct_, xt):
            for dc in range(DC):
                k = orot.next()
                pairs = [(wout[:, m, dc * P:(dc + 1) * P], mt[:, m, :]) for m in range(5)]
                pairs += [(wout[:, 5 + m, dc * P:(dc + 1) * P], oct_[:, m, :]) for m in range(3)]
                MM(psum[k][:], pairs, [wout.b, mt.b, oct_.b], [b_ps[k]])
                STT("dve", xt[:, dc, :], psum[k][:], md[:, 2 * DC + dc:2 * DC + dc + 1], xt[:, dc, :],
                    ALU.mult, ALU.add, [b_ps[k], md.b, xt.b], [xt.b])
            ST(xt, xt[:], xT_v[:, :, i * TT:(i + 1) * TT], xT_dram[i])

        curA = p1b_A(0)
        for i in range(NT):
            xt = xtr.next()
            LD(xt, xt[:], xT_v[:, :, i * TT:(i + 1) * TT], reads=[xT_dram[i]])
            if l == 0:
                bgd_run(bgd_per)
            nxtA = p1b_A(i + 1) if i + 1 < NT else None
            p1b_B(i, curA[0], curA[1], xt)
            curA = nxtA
        pg.barrier()
        ar.reset(pbase)

        dense = (l % 2 == 0)
        li = l // 2
        if dense and cfg.dense_ts:
            ffn_dense_ts(l, li)
            pg.barrier()
            ar.reset(pbase)
            continue
        if (not dense) and cfg.sparse:
            assert l == L - 1, "sparse MoE path assumes the MoE layer is the last layer"
            moe_sparse(l, li)
            pg.barrier()
            ar.reset(pbase)
            continue
        STK = min(1024, S)
        NSB = STK // TT
        dffx = DFF if dense else DFFE
        y_acc = ar.alloc("y_acc", [P, DC, STK], F32)
        h_st = ar.alloc("h_st", [P, DC, STK], BF16)
        wsets = Rot([(ar.alloc(f"wg{i}", [P, DC, FB], BF16), ar.alloc(f"wu{i}", [P, DC, FB], BF16),
                      ar.alloc(f"wd{i}", [P, FB // P, D], BF16)) for i in range(2)])
        xtr = Rot([ar.alloc(f"xt{i}", [P, DC, TT], F32) for i in range(2)])
        stage.items = stage.items[:2] + [ar.alloc(f"stgx{i}", [P, 2048], F32) for i in range(2)]
        sq = ar.alloc("sq", [P, DC, TT], BF16)
        rt = ar.alloc("rt", [P, TT], F32)
        rstd = ar.alloc("rstd", [P, TT], F32)
        abr = Rot([ar.alloc(f"ab{i}", [P, FB // P, TT], BF16) for i in range(2)])
        sgr = Rot([ar.alloc(f"sg{i}", [P, TT], BF16) for i in range(2)])
        ttr = Rot([ar.alloc(f"tt{i}", [P, TT], BF16) for i in range(2)])
        if not dense:
            rwt = ar.alloc("rwt", [P, DC, E], F32)
            LD(rwt, rwt[:], rw[li].rearrange("(c p) e -> p c e", p=P))
            rbb = ar.alloc("rbb", [P, E], F32)
            LD(rbb, rbb[:], rb[li].partition_broadcast(P))
            sel = ar.alloc("sel", [E, E * P], F32)
            LD(sel, sel[:], sel_in)
            combT = ar.alloc("combT", [E, STK], F32)
            cwb = [ar.alloc(f"cwb{t}", [P, TT], F32) for t in range(NSB)]
            rsm = Rot([ar.alloc(f"rsm{i}", [P, 64], F32) for i in range(2)])
        grot = Rot([0, 1])
        urot = Rot([2, 3])
        yrot = Rot([4, 5])
        gate_f = lambda dc: md[:, 5 * DC + dc:5 * DC + dc + 1]
        shift_f = lambda c: md[:, 3 * DC + c:3 * DC + c + 1]
        for sbi in range(S // STK):
            MSET("pool", y_acc[:], 0.0, [y_acc.b])
            for t in range(NSB):
                i = sbi * NSB + t
                xt = xtr.next()
                LD(xt, xt[:], xT_v[:, :, i * TT:(i + 1) * TT], reads=[xT_dram[i]])
                norm_tile(xt, sq, rt, rstd, 6, gscf[l], shift_f,
                          HOut(h_st, lambda c, t=t: h_st[:, c, t * TT:(t + 1) * TT]), not dense)
                if dense:
                    continue
                for blk in range(4):
                    r = rsm.next()
                    lgt = r[:, 0:E]
                    eq1 = r[:, 8:8 + E]
                    lg2 = r[:, 16:16 + E]
                    eq2 = r[:, 24:24 + E]
                    cmb = r[:, 32:32 + E]
                    m1 = r[:, 40:41]
                    m2_ = r[:, 41:42]
                    dd = r[:, 42:43]
                    ex = r[:, 43:44]
                    den = r[:, 44:45]
                    w1 = r[:, 45:46]
                    w2 = r[:, 46:47]
                    MM(psum[7][:, 0:E], [(xt[:, c, blk * P:(blk + 1) * P], rwt[:, c, :]) for c in range(DC)],
                       [xt.b, rwt.b], [b_ps[7]])
                    TTo("dve", lgt, psum[7][:, 0:E], rbb[:], ALU.add, [b_ps[7], rbb.b], [r.b])
                    pg.op("dve", lambda e, m1=m1, lgt=lgt: e.tensor_reduce(out=m1, in_=lgt, axis=AX.X, op=ALU.max),
                          [r.b], [r.b])
                    TS("dve", eq1, lgt, m1, ALU.is_equal, [r.b], [r.b])
                    STT("dve", lg2, eq1, -1e30, lgt, ALU.mult, ALU.add, [r.b], [r.b])
                    pg.op("dve", lambda e, m2_=m2_, lg2=lg2: e.tensor_reduce(out=m2_, in_=lg2, axis=AX.X, op=ALU.max),
                          [r.b], [r.b])
                    TS("dve", eq2, lg2, m2_, ALU.is_equal, [r.b], [r.b])
                    TTo("dve", dd, m2_, m1, ALU.subtract, [r.b], [r.b])
                    ACT(ex, dd, AF.Exp, [r.b], [r.b])
                    TS("dve", den, ex, 1.0, ALU.add, [r.b], [r.b])
                    RECIP(w1, den, [r.b], [r.b])
                    TTo("dve", w2, ex, w1, ALU.mult, [r.b], [r.b])
                    TS("dve", cmb, eq1, w1, ALU.mult, [r.b], [r.b])
                    STT("dve", cmb, eq2, w2, cmb, ALU.mult, ALU.add, [r.b], [r.b])
                    pg.op("pe", lambda e, cmb=cmb, blk=blk: e.transpose(psum[6][0:E, blk * P:(blk + 1) * P], cmb, ident[:]),
                          [r.b, ident.b], [b_ps[6]])
                CP("act", combT[:, t * TT:(t + 1) * TT], psum[6][0:E, :], [b_ps[6]], [combT.b])

            nfb = (dffx + FB - 1) // FB
            seq = [(e_, fb) for e_ in (range(1) if dense else range(E)) for fb in range(nfb)]

            def prefetch(n):
                e_, fb = seq[n]
                wg_, wu_, wd_ = wsets.items[n % 2]
                f0 = fb * FB
                fw = min(FB, dffx - f0)
                if dense:
                    LD(wg_, wg_[:, :, 0:fw], FWg[li][:, f0:f0 + fw].rearrange("(c p) f -> p c f", p=P), reads=[fq_d])
                    LD(wu_, wu_[:, :, 0:fw], FWu[li][:, f0:f0 + fw].rearrange("(c p) f -> p c f", p=P), reads=[fq_d])
                    LD(wd_, wd_[:, 0:fw // P, :], FWd[li][f0:f0 + fw, :].rearrange("(j p) d -> p j d", p=P),
                       reads=[fq_d])
                    return
                sg_, su_, sd_ = mwg[li, e_], mwu[li, e_], mwd[li, e_]
                cpp = max(1, 2048 // fw)
                for dst, src in ((wg_, sg_), (wu_, su_)):
                    sv = src[:, f0:f0 + fw].rearrange("(c p) f -> p c f", p=P)
                    for c0 in range(0, DC, cpp):
                        c1 = min(DC, c0 + cpp)
                        load_cast(dst, dst[:, c0:c1, 0:fw], sv[:, c0:c1, :], (c1 - c0, fw), eng="act")
                dv = sd_[f0:f0 + fw, :].rearrange("(j p) d -> p j d", p=P)
                nj = fw // P
                jpp = max(1, 2048 // D)
                for j0 in range(0, nj, jpp):
                    j1 = min(nj, j0 + jpp)
                    load_cast(wd_, wd_[:, j0:j1, :], dv[:, j0:j1, :], (j1 - j0, D), eng="act")

            def gu_step(n, t):
                e_, fb = seq[n]
                wg_, wu_, wd_ = wsets.items[n % 2]
                f0 = fb * FB
                fw = min(FB, dffx - f0)
                nfc = fw // P
                hs = lambda c, t=t: h_st[:, c, t * TT:(t + 1) * TT]
                if (not dense) and fb == 0:
                    MM(psum[7][:], [(sel[:, e_ * P:(e_ + 1) * P], combT[:, t * TT:(t + 1) * TT])],
                       [sel.b, combT.b], [b_ps[7]])
                    CP("act", cwb[t][:], psum[7][:], [b_ps[7]], [cwb[t].b])
                ab = abr.next()
                for fc in range(nfc):
                    kg = grot.next()
                    ku = urot.next()
                    MM(psum[kg][:], [(wg_[:, c, fc * P:(fc + 1) * P], hs(c)) for c in range(DC)],
                       [wg_.b, h_st.b], [b_ps[kg]])
                    MM(psum[ku][:], [(wu_[:, c, fc * P:(fc + 1) * P], hs(c)) for c in range(DC)],
                       [wu_.b, h_st.b], [b_ps[ku]])
                    sg = sgr.next()
                    ACT(sg[:], psum[kg][:], AF.Silu, [b_ps[kg]], [sg.b])
                    if dense:
                        TTo("dve", ab[:, fc, :], psum[ku][:], sg[:], ALU.mult, [b_ps[ku], sg.b], [ab.b])
                    else:
                        tt_ = ttr.next()
                        TTo("dve", tt_[:], psum[ku][:], sg[:], ALU.mult, [b_ps[ku], sg.b], [tt_.b])
                        TTo("dve", ab[:, fc, :], tt_[:], cwb[t][:], ALU.mult, [tt_.b, cwb[t].b], [ab.b])
                return ab

            def down_step(n, t, ab):
                e_, fb = seq[n]
                wg_, wu_, wd_ = wsets.items[n % 2]
                f0 = fb * FB
                fw = min(FB, dffx - f0)
                nfc = fw // P
                for dc in range(DC):
                    ky = yrot.next()
                    MM(psum[ky][:], [(wd_[:, fc, dc * P:(dc + 1) * P], ab[:, fc, :]) for fc in range(nfc)],
                       [wd_.b, ab.b], [b_ps[ky]])
                    ya = y_acc[:, dc, t * TT:(t + 1) * TT]
                    TTo("dve", ya, psum[ky][:], ya, ALU.add, [b_ps[ky], y_acc.b], [y_acc.b])

            steps = [(n, t) for n in range(len(seq)) for t in range(NSB)]
            prefetch(0)
            if len(seq) > 1:
                prefetch(1)
            abc = gu_step(*steps[0])
            for k, (n, t) in enumerate(steps):
                if k % 6 == 0:
                    bgd_run(bgd_per)
                abn = gu_step(*steps[k + 1]) if k + 1 < len(steps) else None
                down_step(n, t, abc)
                abc = abn
                if t == NSB - 1 and n + 2 < len(seq):
                    prefetch(n + 2)
            for t in range(NSB):
                i = sbi * NSB + t
                xt = xtr.next()
                LD(xt, xt[:], xT_v[:, :, i * TT:(i + 1) * TT], reads=[xT_dram[i]])
                for dc in range(DC):
                    STT("dve", xt[:, dc, :], y_acc[:, dc, t * TT:(t + 1) * TT], gate_f(dc), xt[:, dc, :],
                        ALU.mult, ALU.add, [y_acc.b, md.b, xt.b], [xt.b])
                ST(xt, xt[:], xT_v[:, :, i * TT:(i + 1) * TT], xT_dram[i])
        bgd_run(len(bgd_jobs))
        pg.barrier()
        ar.reset(pbase)
        stage.items = stage.items[:2]

    comb = bool(moe_last and cfg.sparse)
    gb = ar.alloc("gb", [P, D], F32)
    LD(gb, gb[:], fng.partition_broadcast(P))
    xtr = Rot([ar.alloc(f"xt{i}", [P, DC, TT], F32) for i in range(2)])
    otr = Rot([ar.alloc(f"ot{i}", [P, D], F32) for i in range(2)])
    sqj = ar.alloc("sqj", [P, D], F32)
    ssr = Rot([ar.alloc(f"ssr{i}", [P, 8], F32) for i in range(2)])
    if comb:
        mdl = mod[L - 1]
        gate_b = ar.alloc("gate_b", [P, D], F32)
        dgr = Rot([ar.alloc(f"dgt{i}", [P, P], F32) for i in range(2)])
        for c in range(DC):
            dgt = dgr.next()
            TS("dve", dgt[:], ident[:], mdl[:, 5 * DC + c:5 * DC + c + 1], ALU.mult, [ident.b, mdl.b], [dgt.b])
            MM(psum[c // 4][:, (c % 4) * P:(c % 4 + 1) * P], [(ones_f[:], dgt[:])], [ones_f.b, dgt.b], [b_ps[c // 4]])
        CP("act", gate_b[:, 0:512], psum[0][:], [b_ps[0]], [gate_b.b])
        CP("dve", gate_b[:, 512:1024], psum[1][:], [b_ps[1]], [gate_b.b])
        y1r = Rot([ar.alloc(f"y1{i}", [P, D], F32) for i in range(4)])
        y2r = Rot([ar.alloc(f"y2{i}", [P, D], F32) for i in range(4)])
        xsr = Rot([ar.alloc(f"xs{i}", [P, D], F32) for i in range(2)])
    pf_state = {"nblk": 0}

    def pf_A(i, b, xt):
            gbk = i * 4 + b
            o = pf_state["nblk"] % 2
            pf_state["nblk"] += 1
            kb = [4 + 2 * o, 5 + 2 * o]
            ot = otr.next()
            s_ = ssr.next()

            def tp(e, xt=xt, b=b, kb=kb):
                ins = None
                for c in range(DC):
                    bank = kb[c // 4]
                    ins = e.transpose(psum[bank][:, (c % 4) * P:(c % 4 + 1) * P],
                                      xt[:, c, b * P:(b + 1) * P], ident[:])
                return ins
            pg.op("pe", tp, [xt.b, ident.b], [b_ps[kb[0]], b_ps[kb[1]]])
            if comb:
                y1, y2 = pf_gath.pop(gbk)
                xs = xsr.next()
                TS("dve", y1[:], y1[:], w12[:, 2 * gbk:2 * gbk + 1], ALU.mult, [y1.b, w12.b], [y1.b])
                STT("dve", y1[:], y2[:], w12[:, 2 * gbk + 1:2 * gbk + 2], y1[:], ALU.mult, ALU.add,
                    [y1.b, y2.b, w12.b], [y1.b])
                TTo("dve", y1[:], y1[:], gate_b[:], ALU.mult, [y1.b, gate_b.b], [y1.b])
                TTo("dve", xs[:, 0:512], psum[kb[0]][:], y1[:, 0:512], ALU.add, [b_ps[kb[0]], y1.b], [xs.b])
                TTo("dve", xs[:, 512:1024], psum[kb[1]][:], y1[:, 512:1024], ALU.add, [b_ps[kb[1]], y1.b], [xs.b])
                pg.op("act", lambda e, s_=s_, xs=xs: e.activation(out=sqj[:], in_=xs[:], func=AF.Square,
                                                                  accum_out=s_[:, 2:3]),
                      [xs.b], [sqj.b, s_.b])
            else:
                def sqf(e, s_=s_, kb=kb):
                    e.activation(out=sqj[:, 0:512], in_=psum[kb[0]][:], func=AF.Square, accum_out=s_[:, 0:1])
                    return e.activation(out=sqj[:, 512:1024], in_=psum[kb[1]][:], func=AF.Square,
                                        accum_out=s_[:, 1:2])
                pg.op("act", sqf, [b_ps[kb[0]], b_ps[kb[1]]], [sqj.b, s_.b])
                TTo("dve", s_[:, 2:3], s_[:, 0:1], s_[:, 1:2], ALU.add, [s_.b], [s_.b])
            ACT(s_[:, 3:4], s_[:, 2:3], AF.Sqrt, [s_.b, eps_t.b], [s_.b], bias=eps1, scale=1.0 / D)
            return (s_, ot, kb, (xs if comb else None))

    def pf_B(i, b, st):
            s_, ot, kb, xs = st
            RECIP(s_[:, 4:5], s_[:, 3:4], [s_.b], [s_.b])
            if comb:
                STT("dve", ot[:], xs[:], s_[:, 4:5], gb[:], ALU.mult, ALU.mult, [xs.b, s_.b, gb.b], [ot.b])
            else:
                def nrm(e, s_=s_, kb=kb, ot=ot):
                    e.scalar_tensor_tensor(out=ot[:, 0:512], in0=psum[kb[0]][:], scalar=s_[:, 4:5],
                                           in1=gb[:, 0:512], op0=ALU.mult, op1=ALU.mult)
                    return e.scalar_tensor_tensor(out=ot[:, 512:1024], in0=psum[kb[1]][:], scalar=s_[:, 4:5],
                                                  in1=gb[:, 512:1024], op0=ALU.mult, op1=ALU.mult)
                pg.op("dve", nrm, [b_ps[kb[0]], b_ps[kb[1]], s_.b, gb.b], [ot.b])
            r0 = i * TT + b * P
            ev = ST(ot, ot[:], y_out[r0:r0 + P, :], None, q="sp")
            pg.final.append(ev)

    blocks = [(i, b) for i in range(NT) for b in range(4)]
    xts = {}
    pf_gath = {}

    def pf_G(gbk):
        if (not comb) or gbk >= NBLK or gbk in pf_gath:
            return
        y1 = y1r.next()
        y2 = y2r.next()
        for yy, sl_i in ((y1, slot1_i), (y2, slot2_i)):
            pg.dma("pool", [lambda e, yy=yy, sl_i=sl_i, gbk=gbk: e.indirect_dma_start(
                out=yy[:], out_offset=None, in_=Y_slots[:, :],
                in_offset=bass.IndirectOffsetOnAxis(ap=sl_i[:, gbk:gbk + 1], axis=0),
                bounds_check=None)],
                yy.b, reads=[sl_i.b] + yslots_d, writes=[yy.b])
        pf_gath[gbk] = (y1, y2)
    for g_ in range(3):
        pf_G(g_)

    def pf_getxt(i):
        if i not in xts:
            xt = xtr.next()
            LD(xt, xt[:], xT_v[:, :, i * TT:(i + 1) * TT], reads=[xT_dram[i]])
            xts[i] = xt
        return xts[i]
    stA = pf_A(0, 0, pf_getxt(0))
    for n, (i, b) in enumerate(blocks):
        pf_G(n + 3)
        if n + 1 < len(blocks):
            i2, b2 = blocks[n + 1]
            stN = pf_A(i2, b2, pf_getxt(i2))
        else:
            stN = None
        pf_B(i, b, stA)
        stA = stN

    pg.emit()
    return nc


def _consts(S, E):
    NT = S // TT
    inv = np.zeros((2, 3, P, TT), np.float32)
    wins = (2, 4, 8, 16)
    for c in range(2):
        for v, i in enumerate((0, min(1, NT - 1), NT - 1)):
            t = np.arange(i * TT, (i + 1) * TT)
            for g in range(2):
                half = wins[2 * c + g] // 2
                hi = np.clip(t + half, 0, S)
                lo = np.clip(t - half, 0, S)
                inv[c, v, g * 64:(g + 1) * 64, :] = (1.0 / (hi - lo).astype(np.float32))[None, :]
    sel = np.zeros((E, E, P), np.float32)
    for e in range(E):
        sel[e, e, :] = 1.0
    NTL = (2 * S) // TT + E
    utri = np.triu(np.ones((P, P), np.float32), k=1)
    rc = np.zeros((P, NTL + 1), np.float32)
    rc[:, :NTL] = (np.arange(NTL, dtype=np.float32) * TT)[None, :]
    rc[:, NTL] = np.arange(P, dtype=np.float32)
    return {"ident": np.eye(P, dtype=np.float32), "invcnt": inv, "sel": sel.reshape(E, E * P),
            "utri": utri, "rc": rc}


def _pc(v, nchunk):
    v = np.asarray(v, np.float32)
    return np.ascontiguousarray(np.swapaxes(v.reshape(v.shape[:-1] + (nchunk, P)), -1, -2))


def make_in_maps(inputs, cfg):
    f = lambda k: np.ascontiguousarray(np.asarray(inputs[k]), dtype=np.float32)
    x = f("x")
    B = x.shape[0]
    c = f("c")
    L = cfg.L
    shared = {
        "ada_w": f("ada_w"),
        "ada_b_pc": _pc(f("ada_b"), 6 * cfg.DC),
        "mixg_pc": _pc(f("mix_norm_g"), cfg.DC),
        "ffng_pc": _pc(f("ffn_norm_g"), cfg.DC),
        "w_in": f("w_in"),
        "pool_w": f("pool_w"),
        "pool_scale": f("pool_scale"),
        "conv_wT": np.ascontiguousarray(np.swapaxes(f("conv_w"), 1, 2)),
        "conv_b_pc": _pc(f("conv_b"), 3),
        "conv_ln_g_pc": _pc(f("conv_ln_g"), 3),
        "conv_ln_b_pc": _pc(f("conv_ln_b"), 3),
        "sgu_ln_g": f("sgu_ln_g"),
        "sgu_ln_b": f("sgu_ln_b"),
        "sgu_wT": np.ascontiguousarray(np.swapaxes(f("sgu_w"), 2, 3)),
        "sgu_b": f("sgu_b").reshape(L, -1),
        "w_out": f("w_out"),
        "ffn_w_gate": f("ffn_w_gate"),
        "ffn_w_up": f("ffn_w_up"),
        "ffn_w_down": f("ffn_w_down"),
        "router_w": f("router_w"),
        "router_b": f("router_b"),
        "moe_w_gate": f("moe_w_gate"),
        "moe_w_up": f("moe_w_up"),
        "moe_w_down": f("moe_w_down"),
        "final_norm_g": f("final_norm_g"),
    }
    shared.update(_consts(cfg.S, cfg.E))
    maps = []
    for b in range(B):
        m = dict(shared)
        m["x"] = x[b]
        m["c_pc"] = _pc(c[b], cfg.DC)
        maps.append(m)
    return maps


def kernel(**inputs):
    x = np.asarray(inputs["x"])
    B, S, D = x.shape
    cfg = Cfg(S=S, D=D, n_layers=int(np.asarray(inputs["w_in"]).shape[0]),
              d_ff=int(np.asarray(inputs["ffn_w_gate"]).shape[2]),
              n_exp=int(np.asarray(inputs["router_w"]).shape[2]),
              d_ffe=int(np.asarray(inputs["moe_w_gate"]).shape[3]))
    in_maps = make_in_maps(inputs, cfg)
    nc = build(cfg)
    res = run_bass_kernel_spmd(nc, in_maps, core_ids=list(range(B)))
    return np.stack([np.asarray(r["y"], dtype=np.float32) for r in res.results], axis=0)
```

```python
import numpy as np
import concourse.bass as bass
import concourse.mybir as mybir
from concourse.bass_utils import run_bass_kernel_spmd

F32 = mybir.dt.float32
BF16 = mybir.dt.bfloat16
AF = mybir.ActivationFunctionType
ALU = mybir.AluOpType
AX = mybir.AxisListType

P = 128
TT = 512


class Buf:
    __slots__ = ("name", "w", "r", "dsem", "dcnt")

    def __init__(self, name):
        self.name = name
        self.w = {}
        self.r = {}
        self.dsem = None
        self.dcnt = 0


class Prog:
    ENGS = ("pe", "act", "dve", "pool", "sp")
    ROT = 20000

    def __init__(self, nc):
        self.nc = nc
        self.streams = {e: [] for e in self.ENGS}
        self.nsem = 0
        self.sem = {e: self._newsem(e) for e in self.ENGS}
        self.cnt = {e: 0 for e in self.ENGS}
        self.known = {e: {} for e in self.ENGS}
        self.final = []
        self.dma_evs = {}
        self.bg = set()

    def _newsem(self, tag):
        self.nsem += 1
        return self.nc.alloc_semaphore(f"s{self.nsem}_{tag}")

    def _collect(self, eng, reads, writes, pwrites=()):
        need = {}
        def add(ev):
            if ev is None:
                return
            s, v = ev
            k = id(s)
            if k not in need or need[k][1] < v:
                need[k] = (s, v)
        for b in reads:
            for ev in b.w.values():
                add(ev)
        for b in writes:
            for ev in b.w.values():
                add(ev)
            for ev in b.r.values():
                add(ev)
        for b in pwrites:
            for ev in b.r.values():
                add(ev)
        out = []
        kn = self.known[eng]
        for k, (s, v) in need.items():
            if kn.get(k, 0) >= v:
                continue
            kn[k] = v
            out.append((s, v))
        return out

    def op(self, eng, fn, reads=(), writes=()):
        waits = self._collect(eng, reads, writes)
        if self.cnt[eng] >= self.ROT:
            self.sem[eng] = self._newsem(eng)
            self.cnt[eng] = 0
        self.cnt[eng] += 1
        ev = (self.sem[eng], self.cnt[eng])
        self.streams[eng].append((fn, waits, (self.sem[eng], 1)))
        for b in reads:
            b.r[id(ev[0])] = ev
        for b in writes:
            b.w[id(ev[0])] = ev
            b.r = {}
        return ev

    def dma(self, q, fns, sb, reads=(), writes=(), pwrites=()):
        waits = self._collect(q, reads, writes, pwrites)
        if sb.dsem is None:
            sb.dsem = self._newsem("d_" + sb.name)
        for i, fn in enumerate(fns):
            self.streams[q].append((fn, waits if i == 0 else [], (sb.dsem, 16)))
        sb.dcnt += 16 * len(fns)
        ev = (sb.dsem, sb.dcnt)
        self.dma_evs[id(sb.dsem)] = ev
        for b in reads:
            b.r[id(ev[0])] = ev
        for b in writes:
            b.w[id(ev[0])] = ev
            b.r = {}
        for b in pwrites:
            b.w[id(ev[0])] = ev
        return ev

    def barrier(self):
        evs = [(self.sem[e], self.cnt[e]) for e in self.ENGS if self.cnt[e] > 0]
        evs += [ev for k, ev in self.dma_evs.items() if k not in self.bg]
        for e in self.ENGS:
            kn = self.known[e]
            waits = []
            for s_, v in evs:
                if kn.get(id(s_), 0) >= v:
                    continue
                kn[id(s_)] = v
                waits.append((s_, v))
            if waits:
                self.streams[e].append((None, waits, None))

    def emit(self):
        nc = self.nc
        engmap = {"pe": "tensor", "act": "scalar", "dve": "vector",
                  "pool": "gpsimd", "sp": "sync"}
        final = self.final
        with nc.Block() as block:
            for e in self.ENGS:
                stream = self.streams[e]
                last = (e == "sp")

                def body(eng, stream=stream, last=last):
                    for fn, waits, inc in stream:
                        for s, v in waits:
                            eng.wait_ge(s, v)
                        if fn is None:
                            continue
                        ins = fn(eng)
                        ins.then_inc(inc[0], inc[1])
                    if last:
                        fin = {}
                        for s, v in final:
                            if id(s) not in fin or fin[id(s)][1] < v:
                                fin[id(s)] = (s, v)
                        for s, v in fin.values():
                            eng.wait_ge(s, v)
                getattr(block, engmap[e])(body)


I32 = mybir.dt.int32
SB_LO = 16512
SB_WORDS = 53200
DIN = 1792
FB = 512
POOL_HALF = ((1, 2), (4, 8))


class Cfg:
    def __init__(self, S=8192, D=1024, n_layers=2, d_ff=2816, n_exp=8, d_ffe=3584):
        self.S = S
        self.D = D
        self.L = n_layers
        self.DFF = d_ff
        self.E = n_exp
        self.DFFE = d_ffe
        self.NT = S // TT
        self.DC = D // P
        self.sparse = True
        self.bgcast = True
        self.dense_ts = True


class T:
    def __init__(self, ap, name):
        self.ap = ap
        self.b = Buf(name)

    def __getitem__(self, k):
        return self.ap[k]


def _prod(xs):
    r = 1
    for v in xs:
        r *= v
    return r


class Arena:
    def __init__(self, big):
        self.big = big
        self.cur = 0
        self.n = 0

    def alloc(self, name, shape, dtype):
        esz = 2 if dtype == BF16 else 4
        n = _prod(shape[1:])
        nw = (n * esz + 3) // 4
        nw = (nw + 7) // 8 * 8
        w0 = self.cur
        self.cur += nw
        assert self.cur <= SB_WORDS, (name, self.cur * 4)
        ap = self.big[0:shape[0], w0:w0 + (n * esz + 3) // 4]
        if dtype != F32:
            ap = ap.bitcast(dtype)
        ap = ap[:, 0:n]
        if len(shape) == 3:
            ap = ap.rearrange("p (a b) -> p a b", a=shape[1])
        elif len(shape) == 4:
            ap = ap.rearrange("p (a b c) -> p a b c", a=shape[1], b=shape[2])
        self.n += 1
        return T(ap, f"{name}{self.n}")

    def mark(self):
        return self.cur

    def reset(self, m):
        self.cur = m


class Rot:
    def __init__(self, items):
        self.items = items
        self.i = 0

    def next(self):
        t = self.items[self.i % len(self.items)]
        self.i += 1
        return t


def build(cfg):
    nc = bass.Bass("TRN2", target_bir_lowering=False)
    S, D, DC, NT, L, E = cfg.S, cfg.D, cfg.DC, cfg.NT, cfg.L, cfg.E
    DFF, DFFE = cfg.DFF, cfg.DFFE
    pg = Prog(nc)

    def din(name, shape):
        return nc.dram_tensor(name, list(shape), F32, kind="ExternalInput").ap()

    nd = (L + 1) // 2
    nm = max(L // 2, 1)
    x_in = din("x", [S, D])
    c_pc = din("c_pc", [P, DC])
    ada_w = din("ada_w", [L, D, 6 * D])
    ada_b = din("ada_b_pc", [L, P, 6 * DC])
    mixg = din("mixg_pc", [L, P, DC])
    ffng = din("ffng_pc", [L, P, DC])
    w_in = din("w_in", [L, D, DIN])
    pool_w = din("pool_w", [L, 4, 64, 64])
    pool_scale = din("pool_scale", [L, 256])
    conv_wT = din("conv_wT", [L, 384, 31])
    conv_b = din("conv_b_pc", [L, P, 3])
    clng = din("conv_ln_g_pc", [L, P, 3])
    clnb = din("conv_ln_b_pc", [L, P, 3])
    slng = din("sgu_ln_g", [L, 384])
    slnb = din("sgu_ln_b", [L, 384])
    sgu_wT = din("sgu_wT", [L, 6, P, P])
    sgu_b = din("sgu_b", [L, 6 * P])
    w_out = din("w_out", [L, D, D])
    fwg = din("ffn_w_gate", [nd, D, DFF])
    fwu = din("ffn_w_up", [nd, D, DFF])
    fwd = din("ffn_w_down", [nd, DFF, D])
    rw = din("router_w", [nm, D, E])
    rb = din("router_b", [nm, E])
    mwg = din("moe_w_gate", [nm, E, D, DFFE])
    mwu = din("moe_w_up", [nm, E, D, DFFE])
    mwd = din("moe_w_down", [nm, E, DFFE, D])
    fng = din("final_norm_g", [D])
    ident_in = din("ident", [P, P])
    invcnt_in = din("invcnt", [2, 3, P, TT])
    sel_in = din("sel", [E, E * P])
    NTL = (2 * S) // TT + E
    NSLOT = NTL * TT
    NFBE = (DFFE + FB - 1) // FB
    NFCE = DFFE // P
    NBLK = S // P
    utri_in = din("utri", [P, P])
    rc_in = din("rc", [P, NTL + 1])
    H_tok = nc.dram_tensor("h_tok", [S, D], BF16).ap()
    H_slots = nc.dram_tensor("h_slots", [NSLOT, D], BF16).ap()
    Y_slots = nc.dram_tensor("y_slots", [NSLOT, D], F32).ap()
    WGU_r = nc.dram_tensor("wgu_r", [E * NFBE * P, 2 * DC * FB], BF16).ap()
    WD_r = nc.dram_tensor("wd_r", [E * P, NFCE * D], BF16).ap()
    wq_d = Buf("wq_d")
    FWg = nc.dram_tensor("fwg_bf", [nd, D, DFF], BF16).ap()
    FWu = nc.dram_tensor("fwu_bf", [nd, D, DFF], BF16).ap()
    FWd = nc.dram_tensor("fwd_bf", [nd, DFF, D], BF16).ap()
    fq_d = Buf("fq_d")
    htok_d = Buf("htok_d")
    hslots_d = Buf("hslots_d")
    yslots_d = [Buf(f"ysl{j}") for j in range(NTL)]
    moe_last = (L % 2 == 0)
    y_out = nc.dram_tensor("y", [S, D], F32, kind="ExternalOutput").ap()
    xT = nc.dram_tensor("xT_scr", [D, S], F32).ap()
    xT_v = xT.rearrange("(c p) t -> p c t", p=P)
    ocd = nc.dram_tensor("oc_scr", [384, S], BF16).ap()
    oc_v = ocd.rearrange("(c p) t -> p c t", p=P)
    xT_dram = [Buf(f"xTd{i}") for i in range(NT)]
    oc_dram = [Buf(f"ocd{i}") for i in range(NT)]

    big = nc.alloc_sbuf_tensor("big", [P, SB_WORDS], F32)
    ar = Arena(big)
    psum = [nc.alloc_psum_tensor(f"ps{i}", [P, 512], F32) for i in range(8)]
    b_ps = [Buf(f"ps{i}") for i in range(8)]

    def MM(out, pairs, reads, writes):
        pairs = list(pairs)

        def fn(e):
            ins = None
            n = len(pairs)
            for i, (l, r) in enumerate(pairs):
                ins = e.matmul(out, l, r, start=(i == 0), stop=(i == n - 1))
            return ins
        pg.op("pe", fn, reads, writes)

    def ACT(out, in_, func, reads, writes, bias=None, scale=None):
        kw = {}
        if bias is not None:
            kw["bias"] = bias
        if scale is not None:
            kw["scale"] = scale
        pg.op("act", lambda e: e.activation(out=out, in_=in_, func=func, **kw), reads, writes)

    def TTo(eng, out, in0, in1, op, reads, writes):
        pg.op(eng, lambda e: e.tensor_tensor(out=out, in0=in0, in1=in1, op=op), reads, writes)

    def TS(eng, out, in0, s1, op0, reads, writes, s2=0.0, op1=ALU.add):
        pg.op(eng, lambda e: e.tensor_scalar(out=out, in0=in0, scalar1=s1, scalar2=s2, op0=op0, op1=op1),
              reads, writes)

    def STT(eng, out, in0, scalar, in1, op0, op1, reads, writes):
        pg.op(eng, lambda e: e.scalar_tensor_tensor(out=out, in0=in0, scalar=scalar, in1=in1,
                                                    op0=op0, op1=op1), reads, writes)

    def CP(eng, out, in_, reads, writes):
        if eng == "act":
            pg.op("act", lambda e: e.copy(out=out, in_=in_), reads, writes)
        else:
            pg.op(eng, lambda e: e.tensor_copy(out=out, in_=in_), reads, writes)

    def MSET(eng, ap, val, writes):
        pg.op(eng, lambda e: e.memset(ap, val), (), writes)

    def RECIP(out, in_, reads, writes):
        pg.op("dve", lambda e: e.reciprocal(out=out, in_=in_), reads, writes)

    def LD(dst, dst_ap, src_ap, reads=(), q="sp"):
        return pg.dma(q, [lambda e: e.dma_start(out=dst_ap, in_=src_ap)], dst.b, reads=reads, writes=[dst.b])

    def ST(src, src_ap, dst_ap, dbuf, q="pool"):
        return pg.dma(q, [lambda e: e.dma_start(out=dst_ap, in_=src_ap)], src.b, reads=[src.b],
                      writes=([dbuf] if dbuf is not None else []))

    ident = ar.alloc("ident", [P, P], F32)
    ones_bf = ar.alloc("ones_bf", [P, P], BF16)
    ones_f = ar.alloc("ones_f", [P, P], F32)
    eps_t = ar.alloc("eps", [P, 8], F32)
    cond = ar.alloc("cond", [P, DC], F32)
    mod = [ar.alloc(f"mod{l}", [P, 6 * DC], F32) for l in range(L)]
    gscm = [ar.alloc(f"gscm{l}", [P, DC], F32) for l in range(L)]
    gscf = [ar.alloc(f"gscf{l}", [P, DC], F32) for l in range(L)]
    stage = Rot([ar.alloc(f"stg{i}", [P, 2048], F32) for i in range(2)])
    LD(ident, ident[:], ident_in)
    identb = ar.alloc("identb", [P, P], BF16)
    CP("dve", identb[:], ident[:], [ident.b], [identb.b])
    MSET("pool", ones_bf[:], 1.0, [ones_bf.b])
    MSET("pool", ones_f[:], 1.0, [ones_f.b])
    MSET("pool", eps_t[:], 1e-6, [eps_t.b])
    eps1 = eps_t[:, 0:1]

    def load_cast(dst, dst_ap, src_ap, fshape, eng="pool"):
        stg = stage.next()
        n = _prod(fshape)
        assert n <= 2048
        sv = stg[:, 0:n]
        if len(fshape) == 2:
            sv = sv.rearrange("p (a b) -> p a b", a=fshape[0])
        LD(stg, sv, src_ap)
        CP(eng, dst_ap, sv, [stg.b], [dst.b])

    slot1_i = ar.alloc("slot1_i", [P, NBLK], I32)
    slot2_i = ar.alloc("slot2_i", [P, NBLK], I32)
    w12 = ar.alloc("w12", [P, NBLK * 2], F32)
    pbase = ar.mark()

    wc_jobs = []

    def wc_build(li):
        for e_ in range(E):
            for fb in range(NFBE):
                f0 = fb * FB
                fw = min(FB, DFFE - f0)
                r0 = (e_ * NFBE + fb) * P
                rows = WGU_r[r0:r0 + P, :]
                cpp = max(1, 2048 // fw)
                for gu, src in ((0, mwg[li, e_]), (1, mwu[li, e_])):
                    sv = src[:, f0:f0 + fw].rearrange("(c p) f -> p c f", p=P)
                    for c0 in range(0, DC, cpp):
                        c1 = min(DC, c0 + cpp)
                        o0 = gu * DC * FB + c0 * FB
                        dst = rows[:, o0:o0 + (c1 - c0) * FB].rearrange("p (a b) -> p a b", a=c1 - c0)[:, :, 0:fw]
                        wc_jobs.append((sv[:, c0:c1, :], dst, c1 - c0, fw))
            dv = mwd[li, e_].rearrange("(j p) d -> p j d", p=P)
            rows = WD_r[e_ * P:(e_ + 1) * P, :].rearrange("p (j d) -> p j d", d=D)
            jpp = max(1, 2048 // D)
            for j0 in range(0, NFCE, jpp):
                j1 = min(NFCE, j0 + jpp)
                wc_jobs.append((dv[:, j0:j1, :], rows[:, j0:j1, :], j1 - j0, D))
    wc_state = {"eng": Rot(["act", "dve"])}

    def wc_run(k):
        for _ in range(k):
            if not wc_jobs:
                return
            src3, dst3, a, w = wc_jobs.pop(0)
            stg = wc_state["stg"].next()
            ob = wc_state["out"].next()
            n = a * w
            s3 = stg[:, 0:n].rearrange("p (a b) -> p a b", a=a)
            o3 = ob[:, 0:n].rearrange("p (a b) -> p a b", a=a)
            LD(stg, s3, src3)
            CP(wc_state["eng"].next(), o3, s3, [stg.b], [ob.b])
            pg.dma("pool", [lambda e, dst3=dst3, o3=o3: e.dma_start(out=dst3, in_=o3)], ob.b, reads=[ob.b],
                   pwrites=[wq_d])
    bgd_jobs = []
    bgd_buf = Buf("bgd")

    def bgd_build(li):
        for e_ in range(E):
            for fb in range(NFBE):
                f0 = fb * FB
                fw = min(FB, DFFE - f0)
                r0 = (e_ * NFBE + fb) * P
                rows = WGU_r[r0:r0 + P, :]
                for gu, src in ((0, mwg[li, e_]), (1, mwu[li, e_])):
                    sv = src[:, f0:f0 + fw].rearrange("(c p) f -> p c f", p=P)
                    o0 = gu * DC * FB
                    dst = rows[:, o0:o0 + DC * FB].rearrange("p (c f) -> p c f", c=DC)[:, :, 0:fw]
                    bgd_jobs.append((sv, dst))
            dv = mwd[li, e_].rearrange("(j p) d -> p j d", p=P)
            rows = WD_r[e_ * P:(e_ + 1) * P, :].rearrange("p (j d) -> p j d", d=D)
            for j0 in range(0, NFCE, 7):
                j1 = min(NFCE, j0 + 7)
                bgd_jobs.append((dv[:, j0:j1, :], rows[:, j0:j1, :]))

    def bgd_run(k):
        for _ in range(k):
            if not bgd_jobs:
                return
            src, dst = bgd_jobs.pop(0)
            pg.dma("pool", [lambda e, src=src, dst=dst: e.dma_start(out=dst, in_=src)], bgd_buf, pwrites=[wq_d])
            pg.bg.add(id(bgd_buf.dsem))
    if L >= 2 and cfg.sparse:
        if cfg.bgcast:
            bgd_build(0)
        else:
            wc_build(0)
            wc_state["stg"] = Rot(stage.items + [ar.alloc("stgw", [P, 2048], F32)])
            wc_state["out"] = Rot([ar.alloc(f"wcb{i}", [P, 2048], BF16) for i in range(3)])
    pwc = ar.mark()
    wc_total = len(wc_jobs)
    bgd_total = len(bgd_jobs)
    bgd_per = (bgd_total + 3 * NT - 1) // (3 * NT)

    adst = Rot([ar.alloc(f"adst{i}", [P, DC, 512], F32) for i in range(2)])
    adab = ar.alloc("adab", [P, 6 * DC], F32)
    mg = ar.alloc("mg", [P, DC], F32)
    fg = ar.alloc("fg", [P, DC], F32)
    tmp8 = ar.alloc("tmp8", [P, DC], F32)
    modrow = ar.alloc("modrow", [1, 6 * D], F32)
    LD(cond, cond[:], c_pc)
    ACT(cond[:], cond[:], AF.Silu, [cond.b], [cond.b])
    pa_jobs = []

    def pa_layer_begin(l):
        LD(adab, adab[:], ada_b[l])
        LD(mg, mg[:], mixg[l])
        LD(fg, fg[:], ffng[l])

    def pa_block(l, m, n0):
        st = adst.next()
        LD(st, st[:], ada_w[l][:, m * D + n0:m * D + n0 + 512].rearrange("(c p) f -> p c f", p=P))
        k = pa_prot.next()
        MM(psum[k][0:1, :], [(cond[:, c:c + 1], st[:, c, :]) for c in range(DC)],
           [st.b, cond.b], [b_ps[k]])
        CP("act" if (n0 // 512) % 2 == 0 else "dve", modrow[0:1, m * D + n0:m * D + n0 + 512],
           psum[k][0:1, :], [b_ps[k]], [modrow.b])

    def pa_layer_end(l):
        for j in range(6 * DC):
            MM(psum[7][:, j:j + 1], [(modrow[0:1, j * P:(j + 1) * P], ones_f[0:1, 0:1])],
               [modrow.b, ones_f.b], [b_ps[7]])
        TTo("dve", mod[l][:], psum[7][:, 0:6 * DC], adab[:], ALU.add, [b_ps[7], adab.b], [mod[l].b])
        TS("dve", tmp8[:], mod[l][:, DC:2 * DC], 1.0, ALU.add, [mod[l].b], [tmp8.b])
        TTo("dve", gscm[l][:], tmp8[:], mg[:], ALU.mult, [tmp8.b, mg.b], [gscm[l].b])
        TS("dve", tmp8[:], mod[l][:, 4 * DC:5 * DC], 1.0, ALU.add, [mod[l].b], [tmp8.b])
        TTo("dve", gscf[l][:], tmp8[:], fg[:], ALU.mult, [tmp8.b, fg.b], [gscf[l].b])
    pa_prot = Rot([5, 6])
    for l in range(L):
        pa_jobs.append(lambda l=l: pa_layer_begin(l))
        for m in range(6):
            for n0 in range(0, D, 512):
                pa_jobs.append(lambda l=l, m=m, n0=n0: pa_block(l, m, n0))
        pa_jobs.append(lambda l=l: pa_layer_end(l))

    def pa_run(k):
        for _ in range(k):
            if pa_jobs:
                pa_jobs.pop(0)()


    dc_jobs = []
    for li_ in range(nd):
        for src, dst in ((fwg[li_], FWg[li_]), (fwu[li_], FWu[li_])):
            sv = src.rearrange("(c p) f -> p c f", p=P)
            dv_ = dst.rearrange("(c p) f -> p c f", p=P)
            for c in range(DC):
                for f0 in range(0, DFF, 2048):
                    f1 = min(DFF, f0 + 2048)
                    dc_jobs.append((sv[:, c, f0:f1], dv_[:, c, f0:f1], f1 - f0))
        sv = fwd[li_].rearrange("(j p) d -> p j d", p=P)
        dv_ = FWd[li_].rearrange("(j p) d -> p j d", p=P)
        for j0 in range(0, DFF // P, 2):
            j1 = min(DFF // P, j0 + 2)
            dc_jobs.append((sv[:, j0:j1, :], dv_[:, j0:j1, :], (j1 - j0, D)))
    dc_state = {"eng": Rot(["act", "dve"])}

    def dc_run(k):
        for _ in range(k):
            if not dc_jobs:
                return
            src, dst, shp = dc_jobs.pop(0)
            stg = dc_state["stg"].next()
            ob = dc_state["out"].next()
            if isinstance(shp, tuple):
                n = shp[0] * shp[1]
                s3 = stg[:, 0:n].rearrange("p (a b) -> p a b", a=shp[0])
                o3 = ob[:, 0:n].rearrange("p (a b) -> p a b", a=shp[0])
            else:
                s3 = stg[:, 0:shp]
                o3 = ob[:, 0:shp]
            LD(stg, s3, src)
            CP(dc_state["eng"].next(), o3, s3, [stg.b], [ob.b])
            pg.dma("pool", [lambda e, dst=dst, o3=o3: e.dma_start(out=dst, in_=o3)], ob.b, reads=[ob.b],
                   pwrites=[fq_d])

    dc_state["stg"] = Rot([ar.alloc(f"dcs{i}", [P, 2048], F32) for i in range(3)])
    dc_state["out"] = Rot([ar.alloc(f"dco{i}", [P, 2048], BF16) for i in range(3)])
    dc_per = (len(dc_jobs) + NT - 1) // NT
    wc_per0 = (len(wc_jobs) + NT - 1) // NT
    xin = Rot([ar.alloc(f"xin{i}", [P, 4, D], F32) for i in range(2)])
    xtr = Rot([ar.alloc(f"xt{i}", [P, DC, TT], F32) for i in range(2)])
    kk = 0
    for i in range(NT):
        xi = xin.next()
        xt = xtr.next()
        LD(xi, xi[:], x_in[i * TT:(i + 1) * TT, :].rearrange("(b p) d -> p b d", p=P))
        for c in range(DC):
            k = kk % 5
            kk += 1

            def tp(e, xi=xi, c=c, k=k):
                ins = None
                for b in range(4):
                    ins = e.transpose(psum[k][:, b * P:(b + 1) * P], xi[:, b, c * P:(c + 1) * P], ident[:])
                return ins
            pg.op("pe", tp, [xi.b, ident.b], [b_ps[k]])
            CP("act" if c % 2 == 0 else "dve", xt[:, c, :], psum[k][:], [b_ps[k]], [xt.b])
        ST(xt, xt[:], xT_v[:, :, i * TT:(i + 1) * TT], xT_dram[i])
        dc_run(dc_per)
        wc_run(wc_per0)
        pa_run((len(pa_jobs) + (NT - 1 - i) - 1) // max(1, NT - 1 - i) if i < NT - 1 else len(pa_jobs))
    dc_run(len(dc_jobs))
    wc_run(len(wc_jobs))
    pa_run(len(pa_jobs))
    pg.barrier()
    ar.reset(pbase)

    def norm_tile(xt, sq, rt, rstd, ssb, gsc, shift_ap, h_out, want_f32):
        hh = DC // 2
        for q in range(2):
            pg.op("act", lambda e, q=q: e.activation(out=sq[:, q * hh:(q + 1) * hh, :],
                                                     in_=xt[:, q * hh:(q + 1) * hh, :], func=AF.Square),
                  [xt.b], [sq.b])
        MM(psum[ssb][:], [(ones_bf[:], sq[:, c, :]) for c in range(DC)], [ones_bf.b, sq.b], [b_ps[ssb]])
        ACT(rt[:], psum[ssb][:], AF.Sqrt, [b_ps[ssb], eps_t.b], [rt.b], bias=eps1, scale=1.0 / D)
        RECIP(rstd[:], rt[:], [rt.b], [rstd.b])
        for c in range(DC):
            TTo("dve", xt[:, c, :], xt[:, c, :], rstd[:], ALU.mult, [xt.b, rstd.b], [xt.b])
            ACT(h_out(c), xt[:, c, :], AF.Identity, [xt.b, gsc.b], [h_out.T.b],
                bias=shift_ap(c), scale=gsc[:, c:c + 1])
            if want_f32:
                TS("dve", xt[:, c, :], xt[:, c, :], gsc[:, c:c + 1], ALU.mult, [xt.b, gsc.b], [xt.b],
                   s2=shift_ap(c), op1=ALU.add)

    class HOut:
        def __init__(self, T_, fn):
            self.T = T_
            self.fn = fn

        def __call__(self, c):
            return self.fn(c)

    def moe_sparse(l, li):
        md = mod[l]
        NBE = NBLK * E
        shift_f = lambda c: md[:, 3 * DC + c:3 * DC + c + 1]
        v3 = lambda t_: t_[:].rearrange("p (b e) -> p b e", e=E)
        eq1_all = ar.alloc("eq1_all", [P, NBE], F32)
        eq2_all = ar.alloc("eq2_all", [P, NBE], F32)
        mask_bf = ar.alloc("mask_bf", [P, NBE], BF16)
        utri_f = ar.alloc("utri_f", [P, P], F32)
        utri = ar.alloc("utri", [P, P], BF16)
        rc = ar.alloc("rc", [P, NTL + 1], F32)
        LD(utri_f, utri_f[:], utri_in)
        CP("dve", utri[:], utri_f[:], [utri_f.b], [utri.b])
        LD(rc, rc[:], rc_in)
        widx = ar.alloc("widx", [P, NTL * NFBE], I32)
        didx = ar.alloc("didx", [P, NTL], I32)
        m1base = ar.mark()
        rwt = ar.alloc("rwt", [P, DC, E], F32)
        LD(rwt, rwt[:], rw[li].rearrange("(c p) e -> p c e", p=P))
        rbb4 = ar.alloc("rbb4", [P, 4, E], F32)
        for blk in range(4):
            LD(rbb4, rbb4[:, blk, :], rb[li].partition_broadcast(P))
        xtr = Rot([ar.alloc(f"xt{i}", [P, DC, TT], F32) for i in range(2)])
        sq = ar.alloc("sq", [P, DC, TT], BF16)
        rt = ar.alloc("rt", [P, TT], F32)
        rstd = ar.alloc("rstd", [P, TT], F32)
        hbr = Rot([ar.alloc(f"hb{i}", [P, DC, TT], BF16) for i in range(2)])
        htr = Rot([ar.alloc(f"htok{i}", [P, 4, D], BF16) for i in range(2)])
        rsm = Rot([ar.alloc(f"rsm{i}", [P, 96], F32) for i in range(2)])
        trot = Rot([0, 1, 2, 3])
        def m1_norm(i):
            xt = xtr.next()
            hb_ = hbr.next()
            LD(xt, xt[:], xT_v[:, :, i * TT:(i + 1) * TT], reads=[xT_dram[i]])
            norm_tile(xt, sq, rt, rstd, 6, gscf[l], shift_f, HOut(hb_, lambda c, hb_=hb_: hb_[:, c, :]), True)
            return xt, hb_

        def m1_rest(i, xt, hb_):
            htok = htr.next()
            gb0 = i * 4
            r = rsm.next()
            lgt = r[:, 0:4 * E].rearrange("p (b e) -> p b e", e=E)
            lg2 = r[:, 4 * E:8 * E].rearrange("p (b e) -> p b e", e=E)
            m1 = r[:, 64:68]
            m2_ = r[:, 68:72]
            dd = r[:, 72:76]
            ex = r[:, 76:80]
            den = r[:, 80:84]
            eq1 = v3(eq1_all)[:, gb0:gb0 + 4, :]
            eq2 = v3(eq2_all)[:, gb0:gb0 + 4, :]
            w12v = w12[:].rearrange("p (b k) -> p b k", k=2)
            w1 = w12v[:, gb0:gb0 + 4, 0]
            w2 = w12v[:, gb0:gb0 + 4, 1]
            bc = lambda a_: a_.unsqueeze(2).to_broadcast([P, 4, E])
            for blk in range(4):
                MM(psum[7][:, blk * E:(blk + 1) * E],
                   [(xt[:, c, blk * P:(blk + 1) * P], rwt[:, c, :]) for c in range(DC)],
                   [xt.b, rwt.b], [b_ps[7]])
            TTo("dve", lgt, psum[7][:, 0:4 * E].rearrange("p (b e) -> p b e", e=E), rbb4[:], ALU.add,
                [b_ps[7], rbb4.b], [r.b])
            pg.op("dve", lambda e, m1=m1, lgt=lgt: e.tensor_reduce(out=m1, in_=lgt, axis=AX.X, op=ALU.max),
                  [r.b], [r.b])
            TTo("dve", eq1, lgt, bc(m1), ALU.is_equal, [r.b], [eq1_all.b])
            STT("dve", lg2, eq1, -1e30, lgt, ALU.mult, ALU.add, [r.b, eq1_all.b], [r.b])
            pg.op("dve", lambda e, m2_=m2_, lg2=lg2: e.tensor_reduce(out=m2_, in_=lg2, axis=AX.X, op=ALU.max),
                  [r.b], [r.b])
            TTo("dve", eq2, lg2, bc(m2_), ALU.is_equal, [r.b], [eq2_all.b])
            TTo("dve", dd, m2_, m1, ALU.subtract, [r.b], [r.b])
            ACT(ex, dd, AF.Exp, [r.b], [r.b])
            TS("dve", den, ex, 1.0, ALU.add, [r.b], [r.b])
            RECIP(w1, den, [r.b], [w12.b])
            TTo("dve", w2, ex, w1, ALU.mult, [r.b, w12.b], [w12.b])
            TTo("dve", v3(mask_bf)[:, gb0:gb0 + 4, :], eq1, eq2, ALU.add, [eq1_all.b, eq2_all.b], [mask_bf.b])
            for blk in range(4):
                k = trot.next()
                pbf = psum[k][:].bitcast(BF16)

                def tp(e, hb_=hb_, blk=blk, pbf=pbf):
                    ins = None
                    for c in range(DC):
                        ins = e.transpose(pbf[:, c * P:(c + 1) * P], hb_[:, c, blk * P:(blk + 1) * P], identb[:])
                    return ins
                pg.op("pe", tp, [hb_.b, identb.b], [b_ps[k]])
                CP("act" if blk % 2 == 0 else "dve", htok[:, blk, :], pbf[:, 0:D], [b_ps[k]], [htok.b])
            pg.dma("pool", [lambda e, htok=htok, i=i: e.dma_start(
                out=H_tok[i * TT:(i + 1) * TT, :].rearrange("(b p) d -> p b d", p=P), in_=htok[:])],
                htok.b, reads=[htok.b], pwrites=[htok_d])

        cur1 = m1_norm(0)
        for i in range(NT):
            nxt1 = m1_norm(i + 1) if i + 1 < NT else None
            m1_rest(i, cur1[0], cur1[1])
            cur1 = nxt1
        pg.barrier()
        ar.reset(m1base)
        sa = ar.alloc("sa", [P, NBE], F32)
        sb_ = ar.alloc("sb", [P, NBE], F32)
        tot = ar.alloc("tot", [P, NBE], F32)
        slot_all = ar.alloc("slot_all", [P, NBE], F32)
        prod = ar.alloc("prod", [P, NBE], F32)
        sm = ar.alloc("sm", [P, 16 * E], F32)
        ejt = ar.alloc("ejt", [P, 4 * NTL], F32)
        sf = ar.alloc("sf", [P, 2 * NBLK], F32)
        MM(psum[0][:, 0:NBE], [(utri[:], mask_bf[:])], [utri.b, mask_bf.b], [b_ps[0]])
        MM(psum[1][:, 0:NBE], [(ones_bf[:], mask_bf[:])], [ones_bf.b, mask_bf.b], [b_ps[1]])
        CP("dve", tot[:], psum[1][:, 0:NBE], [b_ps[1]], [tot.b])
        CP("dve", sa[:], tot[:], [tot.b], [sa.b])
        cur, nxt = sa, sb_
        sh = 1
        while sh < NBLK:
            CP("dve", v3(nxt)[:, 0:sh, :], v3(cur)[:, 0:sh, :], [cur.b], [nxt.b])
            TTo("dve", v3(nxt)[:, sh:NBLK, :], v3(cur)[:, sh:NBLK, :], v3(cur)[:, 0:NBLK - sh, :], ALU.add,
                [cur.b], [nxt.b])
            cur, nxt = nxt, cur
            sh *= 2
        incl = cur
        boff = nxt
        TTo("dve", boff[:], incl[:], tot[:], ALU.subtract, [incl.b, tot.b], [boff.b])
        n_e = v3(incl)[:, NBLK - 1, :]
        pa = sm[:, 0:E]
        pm = sm[:, E:2 * E]
        pad = sm[:, 2 * E:3 * E]
        st_ = sm[:, 3 * E:4 * E]
        en_ = sm[:, 4 * E:5 * E]
        for e_ in range(E):
            TS("dve", ejt[:, 0:NTL], rc[:, 0:NTL], v3(incl)[:, NBLK - 1, e_:e_ + 1], ALU.is_lt,
               [rc.b, incl.b, ejt.b], [ejt.b])
            pg.op("dve", lambda e, e_=e_: e.tensor_reduce(out=pa[:, e_:e_ + 1], in_=ejt[:, 0:NTL], axis=AX.X, op=ALU.add),
                  [ejt.b], [sm.b])
        TS("dve", pad, pa, float(TT), ALU.mult, [sm.b], [sm.b])
        MSET("dve", st_[:, 0:1], 0.0, [sm.b])
        for e_ in range(1, E):
            TTo("dve", st_[:, e_:e_ + 1], st_[:, e_ - 1:e_], pad[:, e_ - 1:e_], ALU.add, [sm.b], [sm.b])
        TTo("dve", en_, st_, pad, ALU.add, [sm.b], [sm.b])
        for e_ in range(E):
            TS("dve", v3(boff)[:, :, e_], v3(boff)[:, :, e_], st_[:, e_:e_ + 1], ALU.add, [boff.b, sm.b], [boff.b])
        TTo("dve", slot_all[:], psum[0][:, 0:NBE], boff[:], ALU.add, [b_ps[0], boff.b], [slot_all.b])
        for eq_, sl_i, o in ((eq1_all, slot1_i, 0), (eq2_all, slot2_i, NBLK)):
            TTo("dve", prod[:], eq_[:], slot_all[:], ALU.mult, [eq_.b, slot_all.b], [prod.b])
            pg.op("dve", lambda e, o=o: e.tensor_reduce(out=sf[:, o:o + NBLK], in_=v3(prod), axis=AX.X, op=ALU.add),
                  [prod.b], [sf.b])
            CP("dve", sl_i[:], sf[:, o:o + NBLK], [sf.b], [sl_i.b])
        jv = rc[:, 0:NTL]
        iop = rc[:, NTL:NTL + 1]
        ej = ejt[:, 0:NTL]
        tmpj = ejt[:, NTL:2 * NTL]
        basef = ejt[:, 2 * NTL:3 * NTL]
        tmpk = ejt[:, 3 * NTL:4 * NTL]
        MSET("dve", ej, 0.0, [ejt.b])
        for e_ in range(E):
            TS("dve", tmpj, jv, en_[:, e_:e_ + 1], ALU.is_ge, [rc.b, sm.b, ejt.b], [ejt.b])
            TTo("dve", ej, ej, tmpj, ALU.add, [ejt.b], [ejt.b])
        TS("dve", ej, ej, float(E - 1), ALU.min, [ejt.b], [ejt.b])
        TS("dve", basef, ej, float(NFBE * P), ALU.mult, [ejt.b], [ejt.b])
        wv = widx[:].rearrange("p (j f) -> p j f", f=NFBE)
        for fb in range(NFBE):
            TS("dve", tmpk, basef, iop, ALU.add, [ejt.b, rc.b], [ejt.b], s2=float(fb * P), op1=ALU.add)
            CP("dve", wv[:, :, fb], tmpk, [ejt.b], [widx.b])
        TS("dve", tmpk, ej, float(P), ALU.mult, [ejt.b, rc.b], [ejt.b], s2=iop, op1=ALU.add)
        CP("dve", didx[:], tmpk, [ejt.b], [didx.b])
        h2r = Rot([ar.alloc(f"h2{i}", [P, D], BF16) for i in range(4)])
        for gb in range(NBLK):
            h2 = h2r.next()
            LD(h2, h2[:], H_tok[gb * P:(gb + 1) * P, :], reads=[htok_d])
            for sl_i in (slot1_i, slot2_i):
                pg.dma("pool", [lambda e, h2=h2, sl_i=sl_i, gb=gb: e.indirect_dma_start(
                    out=H_slots[:, :], out_offset=bass.IndirectOffsetOnAxis(ap=sl_i[:, gb:gb + 1], axis=0),
                    in_=h2[:], in_offset=None, bounds_check=None)],
                    h2.b, reads=[h2.b, sl_i.b], pwrites=[hslots_d])
        pg.barrier()
        ar.reset(m1base)
        hsr = Rot([ar.alloc(f"hs{i}", [P, 4, D], BF16) for i in range(2)])
        hgr = Rot([ar.alloc(f"hg{i}", [P, DC, TT], BF16) for i in range(2)])
        aT = ar.alloc("aT", [P, NFCE, TT], BF16)
        wd_ = ar.alloc("wdx", [P, NFCE, D], BF16)
        wgur = Rot([ar.alloc(f"wgu{i}", [P, 2, DC, FB], BF16) for i in range(2)])
        sgr = Rot([ar.alloc(f"sg{i}", [P, TT], BF16) for i in range(2)])
        ytr = Rot([ar.alloc(f"yt{i}", [P, D], F32) for i in range(2)])
        grot = Rot([0, 1])
        urot = Rot([2, 3])
        yrot = Rot([4, 5])
        t2rot = Rot([6, 7])
        def t_load(j):
            hs = hsr.next()
            LD(hs, hs[:], H_slots[j * TT:(j + 1) * TT, :].rearrange("(b p) d -> p b d", p=P), reads=[hslots_d])
            return hs

        def t_transpose(j, hs):
            hg = hgr.next()
            for c in range(DC):
                k = t2rot.next()
                pbf = psum[k][:].bitcast(BF16)

                def tp(e, hs=hs, c=c, pbf=pbf):
                    ins = None
                    for b_ in range(4):
                        ins = e.transpose(pbf[:, b_ * P:(b_ + 1) * P], hs[:, b_, c * P:(c + 1) * P], identb[:])
                    return ins
                pg.op("pe", tp, [hs.b, identb.b], [b_ps[k]])
                CP("act" if c % 2 == 0 else "dve", hg[:, c, :], pbf[:, 0:TT], [b_ps[k]], [hg.b])
            return hg

        def t_wd(j):
            pg.dma("pool", [lambda e, j=j: e.indirect_dma_start(
                out=wd_[:].rearrange("p j d -> p (j d)"), out_offset=None, in_=WD_r[:, :],
                in_offset=bass.IndirectOffsetOnAxis(ap=didx[:, j:j + 1], axis=0),
                bounds_check=None)],
                wd_.b, reads=[didx.b, wq_d], writes=[wd_.b])

        wgu_of = {}

        def t_gather(j, fb):
            if j >= NTL:
                return
            wgu = wgur.next()
            wgu_of[(j, fb)] = wgu
            pg.dma("pool", [lambda e, j=j, fb=fb, wgu=wgu: e.indirect_dma_start(
                out=wgu[:].rearrange("p a c f -> p (a c f)"), out_offset=None, in_=WGU_r[:, :],
                in_offset=bass.IndirectOffsetOnAxis(ap=widx[:, j * NFBE + fb:j * NFBE + fb + 1], axis=0),
                bounds_check=None)],
                wgu.b, reads=[widx.b, wq_d], writes=[wgu.b])

        NAH = len(wgur.items)
        order = [(j, fb) for j in range(NTL) for fb in range(NFBE)]
        hs_cur = t_load(0)
        hg_cur = t_transpose(0, hs_cur)
        for q_ in range(min(NAH, len(order))):
            t_gather(*order[q_])
        t_wd(0)
        gi = NAH
        for j in range(NTL):
            hg = hg_cur
            hs_next = t_load(j + 1) if j + 1 < NTL else None
            for fb in range(NFBE):
                f0 = fb * FB
                fw = min(FB, DFFE - f0)
                wgu = wgu_of.pop((j, fb))
                for fc in range(fw // P):
                    kg = grot.next()
                    ku = urot.next()
                    MM(psum[kg][:], [(wgu[:, 0, c, fc * P:(fc + 1) * P], hg[:, c, :]) for c in range(DC)],
                       [wgu.b, hg.b], [b_ps[kg]])
                    MM(psum[ku][:], [(wgu[:, 1, c, fc * P:(fc + 1) * P], hg[:, c, :]) for c in range(DC)],
                       [wgu.b, hg.b], [b_ps[ku]])
                    sg = sgr.next()
                    ACT(sg[:], psum[kg][:], AF.Silu, [b_ps[kg]], [sg.b])
                    TTo("dve", aT[:, f0 // P + fc, :], psum[ku][:], sg[:], ALU.mult, [b_ps[ku], sg.b], [aT.b])
                if gi < len(order):
                    t_gather(*order[gi])
                    gi += 1
            if hs_next is not None:
                hg_cur = t_transpose(j + 1, hs_next)
            for b_ in range(4):
                yt = ytr.next()
                for hf_ in range(2):
                    ky = yrot.next()
                    MM(psum[ky][:], [(aT[:, f, b_ * P:(b_ + 1) * P], wd_[:, f, hf_ * 512:(hf_ + 1) * 512])
                                     for f in range(NFCE)], [aT.b, wd_.b], [b_ps[ky]])
                    CP("act" if hf_ == 0 else "dve", yt[:, hf_ * 512:(hf_ + 1) * 512], psum[ky][:], [b_ps[ky]], [yt.b])
                r0 = j * TT + b_ * P
                pg.dma("sp", [lambda e, yt=yt, r0=r0: e.dma_start(out=Y_slots[r0:r0 + P, :], in_=yt[:])],
                       yt.b, reads=[yt.b], pwrites=[yslots_d[j]])
            if j + 1 < NTL:
                t_wd(j + 1)

    def ffn_dense_ts(l, li):
        md = mod[l]
        NFBD = (DFF + FB - 1) // FB
        NFCD = DFF // P
        gate_f = lambda dc: md[:, 5 * DC + dc:5 * DC + dc + 1]
        shift_f = lambda c: md[:, 3 * DC + c:3 * DC + c + 1]
        wdf = ar.alloc("wdf", [P, NFCD, D], BF16)
        dvv = FWd[li].rearrange("(j p) d -> p j d", p=P)
        for j0 in range(0, NFCD, 6):
            j1 = min(NFCD, j0 + 6)
            LD(wdf, wdf[:, j0:j1, :], dvv[:, j0:j1, :], reads=[fq_d])
        aTd = ar.alloc("aTd", [P, NFCD, TT], BF16)
        wgur = Rot([ar.alloc(f"wgd{i}", [P, 2, DC, FB], BF16) for i in range(3)])
        xt1 = ar.alloc("xtn", [P, DC, TT], F32)
        xr1 = ar.alloc("xr", [P, DC, TT], F32)
        sq = ar.alloc("sq", [P, DC, TT], BF16)
        rt = ar.alloc("rt", [P, TT], F32)
        rstd = ar.alloc("rstd", [P, TT], F32)
        hbr = Rot([ar.alloc(f"hd{i}", [P, DC, TT], BF16) for i in range(2)])
        sgr = Rot([ar.alloc(f"sgd{i}", [P, TT], BF16) for i in range(2)])
        grot = Rot([0, 1])
        urot = Rot([2, 3])
        yrot = Rot([4, 5])
        order = [(i, fb) for i in range(NT) for fb in range(NFBD)]
        wof = {}

        def wload(i, fb):
            w_ = wgur.next()
            wof[(i, fb)] = w_
            f0 = fb * FB
            fw = min(FB, DFF - f0)
            LD(w_, w_[:, 0, :, 0:fw], FWg[li][:, f0:f0 + fw].rearrange("(c p) f -> p c f", p=P), reads=[fq_d])
            LD(w_, w_[:, 1, :, 0:fw], FWu[li][:, f0:f0 + fw].rearrange("(c p) f -> p c f", p=P), reads=[fq_d])

        def dnorm(i):
            h = hbr.next()
            LD(xt1, xt1[:], xT_v[:, :, i * TT:(i + 1) * TT], reads=[xT_dram[i]])
            norm_tile(xt1, sq, rt, rstd, 6, gscf[l], shift_f, HOut(h, lambda c, h=h: h[:, c, :]), False)
            return h
        NAH = len(wgur.items)
        for q_ in range(min(NAH, len(order))):
            wload(*order[q_])
        gi = NAH
        hcur = dnorm(0)
        for i in range(NT):
            if l == 0:
                bgd_run(bgd_per)
            h = hcur
            LD(xr1, xr1[:], xT_v[:, :, i * TT:(i + 1) * TT], reads=[xT_dram[i]])
            for fb in range(NFBD):
                f0 = fb * FB
                fw = min(FB, DFF - f0)
                w_ = wof.pop((i, fb))
                for fc in range(fw // P):
                    kg = grot.next()
                    ku = urot.next()
                    MM(psum[kg][:], [(w_[:, 0, c, fc * P:(fc + 1) * P], h[:, c, :]) for c in range(DC)],
                       [w_.b, h.b], [b_ps[kg]])
                    MM(psum[ku][:], [(w_[:, 1, c, fc * P:(fc + 1) * P], h[:, c, :]) for c in range(DC)],
                       [w_.b, h.b], [b_ps[ku]])
                    sg = sgr.next()
                    ACT(sg[:], psum[kg][:], AF.Silu, [b_ps[kg]], [sg.b])
                    TTo("dve", aTd[:, f0 // P + fc, :], psum[ku][:], sg[:], ALU.mult, [b_ps[ku], sg.b], [aTd.b])
                if gi < len(order):
                    wload(*order[gi])
                    gi += 1
            for dc in range(DC):
                ky = yrot.next()
                MM(psum[ky][:], [(wdf[:, f, dc * P:(dc + 1) * P], aTd[:, f, :]) for f in range(NFCD)],
                   [wdf.b, aTd.b], [b_ps[ky]])
                STT("dve", xr1[:, dc, :], psum[ky][:], gate_f(dc), xr1[:, dc, :], ALU.mult, ALU.add,
                    [b_ps[ky], md.b, xr1.b], [xr1.b])
            ST(xr1, xr1[:], xT_v[:, :, i * TT:(i + 1) * TT], xT_dram[i])
            hcur = dnorm(i + 1) if i + 1 < NT else None
        bgd_run(len(bgd_jobs))

    for l in range(L):
        md = mod[l]
        a_all = ar.alloc("a_all", [P, 2, S + 32], BF16)
        glu_all = ar.alloc("glu_all", [P, 3, S + 32], BF16)
        for tl in (a_all, glu_all):
            MSET("pool", tl[:, :, 0:16], 0.0, [tl.b])
            MSET("pool", tl[:, :, S + 16:S + 32], 0.0, [tl.b])
        p1base = ar.mark()
        win = ar.alloc("win", [P, DC, DIN], BF16)
        for c in range(DC):
            load_cast(win, win[:, c, :], w_in[l][c * P:(c + 1) * P, :], (DIN,))
        lngb = ar.alloc("lngb", [P, 384], F32)
        lnbb = ar.alloc("lnbb", [P, 384], F32)
        LD(lngb, lngb[:], slng[l].partition_broadcast(P))
        LD(lnbb, lnbb[:], slnb[l].partition_broadcast(P))
        swT = ar.alloc("swT", [P, 6, P], BF16)
        load_cast(swT, swT[:], sgu_wT[l].rearrange("h q p -> q h p"), (6, P))
        biasT = ar.alloc("biasT", [P, 3, P], F32)
        for j in range(3):
            for hh in range(2):
                hd = 2 * j + hh
                LD(biasT, biasT[hh * 64:(hh + 1) * 64, j, :], sgu_b[l, hd * P:(hd + 1) * P].partition_broadcast(64))
        xtr = Rot([ar.alloc(f"xt{i}", [P, DC, TT], F32) for i in range(1)])
        sq = ar.alloc("sq", [P, DC, TT], BF16)
        hb = Rot([ar.alloc(f"h{i}", [P, DC, TT], BF16) for i in range(2)])
        rt = ar.alloc("rt", [P, TT], F32)
        rstd = ar.alloc("rstd", [P, TT], F32)
        sgr = Rot([ar.alloc(f"sg{i}", [P, TT], F32) for i in range(2)])
        st6 = Rot([ar.alloc(f"st6{i}", [P, 8], F32) for i in range(2)])
        mv = Rot([ar.alloc(f"mv{i}", [P, 8], F32) for i in range(2)])
        vt = Rot([ar.alloc(f"vt{i}", [P, 384], F32) for i in range(2)])
        vn = Rot([ar.alloc(f"vn{i}", [P, 384], BF16) for i in range(3)])
        mxs = Rot([ar.alloc(f"mxs{i}", [P, TT], F32) for i in range(1)])
        octr = Rot([ar.alloc(f"oct{i}", [P, 3, TT], BF16) for i in range(2)])
        pj = Rot([1, 2])
        vpr = Rot([3, 4])
        def p1a_normA(i):
            xt = xtr.next()
            h = hb.next()
            LD(xt, xt[:], xT_v[:, :, i * TT:(i + 1) * TT], reads=[xT_dram[i]])
            hh_ = DC // 2
            for q in range(2):
                pg.op("act", lambda e, q=q, xt=xt: e.activation(out=sq[:, q * hh_:(q + 1) * hh_, :],
                                                               in_=xt[:, q * hh_:(q + 1) * hh_, :], func=AF.Square),
                      [xt.b], [sq.b])
            MM(psum[0][:], [(ones_bf[:], sq[:, c, :]) for c in range(DC)], [ones_bf.b, sq.b], [b_ps[0]])
            ACT(rt[:], psum[0][:], AF.Sqrt, [b_ps[0], eps_t.b], [rt.b], bias=eps1, scale=1.0 / D)
            RECIP(rstd[:], rt[:], [rt.b], [rstd.b])
            return (xt, h)

        def p1a_normB(st, c):
            xt, h = st
            TTo("dve", xt[:, c, :], xt[:, c, :], rstd[:], ALU.mult, [xt.b, rstd.b], [xt.b])
            ACT(h[:, c, :], xt[:, c, :], AF.Identity, [xt.b, gscm[l].b, md.b], [h.b],
                bias=md[:, c:c + 1], scale=gscm[l][:, c:c + 1])

        def p1a_front(i, h, nxt):
            base = 16 + i * TT

            def proj(f, k):
                MM(psum[k][:], [(win[:, c, f * P:(f + 1) * P], h[:, c, :]) for c in range(DC)],
                   [win.b, h.b], [b_ps[k]])
            for f in range(2):
                k = pj.next()
                proj(f, k)
                CP("act", a_all[:, f, base:base + TT], psum[k][:], [b_ps[k]], [a_all.b])
                if nxt is not None:
                    p1a_normB(nxt, f)
            for j in range(3):
                k = pj.next()
                proj(5 + j, k)
                sg = sgr.next()
                ACT(sg[:], psum[k][:], AF.Sigmoid, [b_ps[k]], [sg.b])
                if nxt is not None:
                    p1a_normB(nxt, 2 + 2 * j)
                k = pj.next()
                proj(2 + j, k)
                TTo("dve", glu_all[:, j, base:base + TT], psum[k][:], sg[:], ALU.mult,
                    [b_ps[k], sg.b], [glu_all.b])
                if nxt is not None:
                    p1a_normB(nxt, 3 + 2 * j)

        def p1a_sgu(i, h):
            base = 16 + i * TT

            def proj(f, k):
                MM(psum[k][:], [(win[:, c, f * P:(f + 1) * P], h[:, c, :]) for c in range(DC)],
                   [win.b, h.b], [b_ps[k]])
            def sgu_chain(blk):
                k = vpr.next()
                MM(psum[k][:, 0:384],
                   [(h[:, c, blk * P:(blk + 1) * P], win[:, c, 1408:1792]) for c in range(DC)],
                   [win.b, h.b], [b_ps[k]])
                s6 = st6.next()
                m2 = mv.next()
                v1 = vt.next()
                v2 = vn.next()
                pg.op("dve", lambda e, s6=s6, k=k: e.bn_stats(out=s6[:, 0:6], in_=psum[k][:, 0:384]),
                      [b_ps[k]], [s6.b])
                pg.op("dve", lambda e, s6=s6, m2=m2: e.bn_aggr(out=m2[:, 0:2], in_=s6[:, 0:6]),
                      [s6.b], [m2.b])
                ACT(m2[:, 2:3], m2[:, 1:2], AF.Sqrt, [m2.b, eps_t.b], [m2.b], bias=eps1, scale=1.0)
                RECIP(m2[:, 3:4], m2[:, 2:3], [m2.b], [m2.b])
                TS("dve", v1[:], psum[k][:, 0:384], m2[:, 0:1], ALU.subtract, [b_ps[k], m2.b], [v1.b],
                   s2=m2[:, 3:4], op1=ALU.mult)
                TTo("pool", v1[:], v1[:], lngb[:], ALU.mult, [v1.b, lngb.b], [v1.b])
                TTo("pool", v2[:], v1[:], lnbb[:], ALU.add, [v1.b, lnbb.b], [v2.b])
                return v2

            def sgu_spatial(blk, v2):
                for j in range(3):
                    def sp(e, j=j, blk=blk, v2=v2):
                        ins = None
                        for hh in range(2):
                            hd = 2 * j + hh
                            o = psum[5 + j][hh * 64:(hh + 1) * 64, blk * P:(blk + 1) * P]
                            ins = e.matmul(o, v2[:, hd * 64:(hd + 1) * 64], swT[:, hd, :], start=True, stop=True)
                        return ins
                    pg.op("pe", sp, [v2.b, swT.b], [b_ps[5 + j]])

            v2s = {}
            v2s[0] = sgu_chain(0)
            v2s[1] = sgu_chain(1)
            sgu_spatial(0, v2s[0])
            v2s[2] = sgu_chain(2)
            sgu_spatial(1, v2s[1])
            v2s[3] = sgu_chain(3)
            sgu_spatial(2, v2s[2])
            sgu_spatial(3, v2s[3])
            oct_ = octr.next()
            for j in range(3):
                mx = mxs.next()
                TTo("dve", mx[:].rearrange("p (b q) -> p b q", b=4), psum[5 + j][:].rearrange("p (b q) -> p b q", b=4),
                    biasT[:, j, :].unsqueeze(1).to_broadcast([P, 4, P]), ALU.add, [b_ps[5 + j], biasT.b], [mx.b])
                k = pj.next()
                proj(8 + j, k)
                TTo("dve", oct_[:, j, :], psum[k][:], mx[:], ALU.mult, [b_ps[k], mx.b], [oct_.b])
            ST(oct_, oct_[:], oc_v[:, :, i * TT:(i + 1) * TT], oc_dram[i])

        st0 = p1a_normA(0)
        for c in range(DC):
            p1a_normB(st0, c)
        hcur = st0[1]
        for i in range(NT):
            if l == 0:
                bgd_run(bgd_per)
            nxt = p1a_normA(i + 1) if i + 1 < NT else None
            p1a_front(i, hcur, nxt)
            p1a_sgu(i, hcur)
            hcur = nxt[1] if nxt is not None else None
        pg.barrier()
        ar.reset(p1base)

        wout = ar.alloc("wout", [P, DC, D], BF16)
        for c in range(DC):
            load_cast(wout, wout[:, c, :], w_out[l][c * P:(c + 1) * P, :], (D,))
        wbf = ar.alloc("wbf", [P, 2, P], F32)
        psb = ar.alloc("psb", [P, 2 * P], F32)
        wbd = ar.alloc("wbd", [P, 2, P], BF16)
        wbh = ar.alloc("wbh", [P, 2, P], BF16)
        MSET("pool", wbf[:], 0.0, [wbf.b])
        MSET("pool", wbh[:], 0.0, [wbh.b])
        for c in range(2):
            for g in range(2):
                LD(wbf, wbf[g * 64:(g + 1) * 64, c, g * 64:(g + 1) * 64], pool_w[l, 2 * c + g])
        LD(psb, psb[:], pool_scale[l].partition_broadcast(P))
        TTo("dve", wbd[:], wbf[:], psb[:].rearrange("p (c f) -> p c f", c=2), ALU.mult, [wbf.b, psb.b], [wbd.b])
        CP("dve", wbh[64:128, :, :], wbd[64:128, :, :], [wbd.b], [wbh.b])
        cwT = ar.alloc("cwT", [P, 3, 31], F32)
        LD(cwT, cwT[:], conv_wT[l].rearrange("(c p) k -> p c k", p=P))
        dg = ar.alloc("dg", [P, 93, P], BF16)
        for c in range(3):
            for k in range(31):
                TS("pool" if (k % 2) else "dve", dg[:, c * 31 + k, :], ident[:], cwT[:, c, k:k + 1], ALU.mult,
                   [ident.b, cwT.b], [dg.b])
        cb = ar.alloc("cb", [P, 3], F32)
        lg_ = ar.alloc("lg", [P, 3], F32)
        lb_ = ar.alloc("lb", [P, 3], F32)
        LD(cb, cb[:], conv_b[l])
        LD(lg_, lg_[:], clng[l])
        LD(lb_, lb_[:], clnb[l])
        invc = ar.alloc("invc", [P, 2, 3, TT], F32)
        LD(invc, invc[:], invcnt_in.rearrange("c v p t -> p c v t"))
        xtr = Rot([ar.alloc(f"xt{i}", [P, DC, TT], F32) for i in range(1)])
        mtr = Rot([ar.alloc(f"mt{i}", [P, 5, TT], BF16) for i in range(2)])
        octr = Rot([ar.alloc(f"oct{i}", [P, 3, TT], BF16) for i in range(2)])
        yb = ar.alloc("yb", [P, 3, TT], BF16)
        ysq = ar.alloc("ysq", [P, 3, TT], BF16)
        t1 = Rot([ar.alloc(f"t1{i}", [P, TT], F32) for i in range(1)])
        mm_ = ar.alloc("m", [P, TT], F32)
        msq = ar.alloc("msq", [P, TT], F32)
        var_ = ar.alloc("var", [P, TT], F32)
        crs = ar.alloc("crs", [P, TT], F32)
        tcr = Rot([ar.alloc(f"tc{i}", [P, TT], F32) for i in range(1)])
        yr = Rot([2, 3])
        orot = Rot([6, 7])
        def p1b_A(i):
            base = 16 + i * TT
            vr = 0 if i == 0 else (2 if i == NT - 1 else 1)
            mt = mtr.next()
            oct_ = octr.next()
            LD(oct_, oct_[:], oc_v[:, :, i * TT:(i + 1) * TT], reads=[oc_dram[i]])
            for c in range(2):
                h0, h1 = POOL_HALF[c]
                pairs = []
                for j in range(-h1, h1):
                    w_ = wbd if (-h0 <= j < h0) else wbh
                    pairs.append((w_[:, c, :], a_all[:, c, base + j:base + j + TT]))
                MM(psum[0][:], pairs, [wbd.b, wbh.b, a_all.b], [b_ps[0]])
                MM(psum[1][:], [(wbd[:, c, :], a_all[:, c, base:base + TT])], [wbd.b, a_all.b], [b_ps[1]])
                tt1 = t1.next()
                TTo("dve", tt1[:], psum[0][:], invc[:, c, vr, :], ALU.mult, [b_ps[0], invc.b], [tt1.b])
                TTo("dve", mt[:, c, :], tt1[:], psum[1][:], ALU.subtract, [tt1.b, b_ps[1]], [mt.b])
            for c in range(3):
                k = yr.next()
                MM(psum[k][:], [(dg[:, c * 31 + kk_, :], glu_all[:, c, base + kk_ - 15:base + kk_ - 15 + TT])
                                for kk_ in range(31)], [dg.b, glu_all.b], [b_ps[k]])
                ACT(yb[:, c, :], psum[k][:], AF.Identity, [b_ps[k], cb.b], [yb.b], bias=cb[:, c:c + 1], scale=1.0)
                ACT(ysq[:, c, :], psum[k][:], AF.Square, [b_ps[k], cb.b], [ysq.b], bias=cb[:, c:c + 1], scale=1.0)
            MM(psum[4][:], [(ones_bf[:], yb[:, c, :]) for c in range(3)], [ones_bf.b, yb.b], [b_ps[4]])
            MM(psum[5][:], [(ones_bf[:], ysq[:, c, :]) for c in range(3)], [ones_bf.b, ysq.b], [b_ps[5]])
            ACT(mm_[:], psum[4][:], AF.Identity, [b_ps[4]], [mm_.b], scale=1.0 / 384.0)
            TTo("dve", msq[:], mm_[:], mm_[:], ALU.mult, [mm_.b], [msq.b])
            STT("dve", var_[:], psum[5][:], 1.0 / 384.0, msq[:], ALU.mult, ALU.subtract,
                [b_ps[5], msq.b], [var_.b])
            ACT(var_[:], var_[:], AF.Sqrt, [var_.b, eps_t.b], [var_.b], bias=eps1, scale=1.0)
            RECIP(crs[:], var_[:], [var_.b], [crs.b])
            for c in range(3):
                tc_ = tcr.next()
                TTo("dve", tc_[:], yb[:, c, :], mm_[:], ALU.subtract, [yb.b, mm_.b], [tc_.b])
                TTo("dve", tc_[:], tc_[:], crs[:], ALU.mult, [tc_.b, crs.b], [tc_.b])
                ACT(mt[:, 2 + c, :], tc_[:], AF.Silu, [tc_.b, lg_.b, lb_.b], [mt.b],
                    bias=lb_[:, c:c + 1], scale=lg_[:, c:c + 1])
            return mt, oct_

        def p1b_B(i, mt, oct_, xt):
            for dc in range(DC):
                k = orot.next()
                pairs = [(wout[:, m, dc * P:(dc + 1) * P], mt[:, m, :]) for m in range(5)]
                pairs += [(wout[:, 5 + m, dc * P:(dc + 1) * P], oct_[:, m, :]) for m in range(3)]
                MM(psum[k][:], pairs, [wout.b, mt.b, oct_.b], [b_ps[k]])
                STT("dve", xt[:, dc, :], psum[k][:], md[:, 2 * DC + dc:2 * DC + dc + 1], xt[:, dc, :],
                    ALU.mult, ALU.add, [b_ps[k], md.b, xt.b], [xt.b])
            ST(xt, xt[:], xT_v[:, :, i * TT:(i + 1) * TT], xT_dram[i])

        curA = p1b_A(0)
        for i in range(NT):
            xt = xtr.next()
            LD(xt, xt[:], xT_v[:, :, i * TT:(i + 1) * TT], reads=[xT_dram[i]])
            if l == 0:
                bgd_run(bgd_per)
            nxtA = p1b_A(i + 1) if i + 1 < NT else None
            p1b_B(i, curA[0], curA[1], xt)
            curA = nxtA
        pg.barrier()
        ar.reset(pbase)

        dense = (l % 2 == 0)
        li = l // 2
        if dense and cfg.dense_ts:
            ffn_dense_ts(l, li)
            pg.barrier()
            ar.reset(pbase)
            continue
        if (not dense) and cfg.sparse:
            assert l == L - 1, "sparse MoE path assumes the MoE layer is the last layer"
            moe_sparse(l, li)
            pg.barrier()
            ar.reset(pbase)
            continue
        STK = min(1024, S)
        NSB = STK // TT
        dffx = DFF if dense else DFFE
        y_acc = ar.alloc("y_acc", [P, DC, STK], F32)
        h_st = ar.alloc("h_st", [P, DC, STK], BF16)
        wsets = Rot([(ar.alloc(f"wg{i}", [P, DC, FB], BF16), ar.alloc(f"wu{i}", [P, DC, FB], BF16),
                      ar.alloc(f"wd{i}", [P, FB // P, D], BF16)) for i in range(2)])
        xtr = Rot([ar.alloc(f"xt{i}", [P, DC, TT], F32) for i in range(2)])
        stage.items = stage.items[:2] + [ar.alloc(f"stgx{i}", [P, 2048], F32) for i in range(2)]
        sq = ar.alloc("sq", [P, DC, TT], BF16)
        rt = ar.alloc("rt", [P, TT], F32)
        rstd = ar.alloc("rstd", [P, TT], F32)
        abr = Rot([ar.alloc(f"ab{i}", [P, FB // P, TT], BF16) for i in range(2)])
        sgr = Rot([ar.alloc(f"sg{i}", [P, TT], BF16) for i in range(2)])
        ttr = Rot([ar.alloc(f"tt{i}", [P, TT], BF16) for i in range(2)])
        if not dense:
            rwt = ar.alloc("rwt", [P, DC, E], F32)
            LD(rwt, rwt[:], rw[li].rearrange("(c p) e -> p c e", p=P))
            rbb = ar.alloc("rbb", [P, E], F32)
            LD(rbb, rbb[:], rb[li].partition_broadcast(P))
            sel = ar.alloc("sel", [E, E * P], F32)
            LD(sel, sel[:], sel_in)
            combT = ar.alloc("combT", [E, STK], F32)
            cwb = [ar.alloc(f"cwb{t}", [P, TT], F32) for t in range(NSB)]
            rsm = Rot([ar.alloc(f"rsm{i}", [P, 64], F32) for i in range(2)])
        grot = Rot([0, 1])
        urot = Rot([2, 3])
        yrot = Rot([4, 5])
        gate_f = lambda dc: md[:, 5 * DC + dc:5 * DC + dc + 1]
        shift_f = lambda c: md[:, 3 * DC + c:3 * DC + c + 1]
        for sbi in range(S // STK):
            MSET("pool", y_acc[:], 0.0, [y_acc.b])
            for t in range(NSB):
                i = sbi * NSB + t
                xt = xtr.next()
                LD(xt, xt[:], xT_v[:, :, i * TT:(i + 1) * TT], reads=[xT_dram[i]])
                norm_tile(xt, sq, rt, rstd, 6, gscf[l], shift_f,
                          HOut(h_st, lambda c, t=t: h_st[:, c, t * TT:(t + 1) * TT]), not dense)
                if dense:
                    continue
                for blk in range(4):
                    r = rsm.next()
                    lgt = r[:, 0:E]
                    eq1 = r[:, 8:8 + E]
                    lg2 = r[:, 16:16 + E]
                    eq2 = r[:, 24:24 + E]
                    cmb = r[:, 32:32 + E]
                    m1 = r[:, 40:41]
                    m2_ = r[:, 41:42]
                    dd = r[:, 42:43]
                    ex = r[:, 43:44]
                    den = r[:, 44:45]
                    w1 = r[:, 45:46]
                    w2 = r[:, 46:47]
                    MM(psum[7][:, 0:E], [(xt[:, c, blk * P:(blk + 1) * P], rwt[:, c, :]) for c in range(DC)],
                       [xt.b, rwt.b], [b_ps[7]])
                    TTo("dve", lgt, psum[7][:, 0:E], rbb[:], ALU.add, [b_ps[7], rbb.b], [r.b])
                    pg.op("dve", lambda e, m1=m1, lgt=lgt: e.tensor_reduce(out=m1, in_=lgt, axis=AX.X, op=ALU.max),
                          [r.b], [r.b])
                    TS("dve", eq1, lgt, m1, ALU.is_equal, [r.b], [r.b])
                    STT("dve", lg2, eq1, -1e30, lgt, ALU.mult, ALU.add, [r.b], [r.b])
                    pg.op("dve", lambda e, m2_=m2_, lg2=lg2: e.tensor_reduce(out=m2_, in_=lg2, axis=AX.X, op=ALU.max),
                          [r.b], [r.b])
                    TS("dve", eq2, lg2, m2_, ALU.is_equal, [r.b], [r.b])
                    TTo("dve", dd, m2_, m1, ALU.subtract, [r.b], [r.b])
                    ACT(ex, dd, AF.Exp, [r.b], [r.b])
                    TS("dve", den, ex, 1.0, ALU.add, [r.b], [r.b])
                    RECIP(w1, den, [r.b], [r.b])
                    TTo("dve", w2, ex, w1, ALU.mult, [r.b], [r.b])
                    TS("dve", cmb, eq1, w1, ALU.mult, [r.b], [r.b])
                    STT("dve", cmb, eq2, w2, cmb, ALU.mult, ALU.add, [r.b], [r.b])
                    pg.op("pe", lambda e, cmb=cmb, blk=blk: e.transpose(psum[6][0:E, blk * P:(blk + 1) * P], cmb, ident[:]),
                          [r.b, ident.b], [b_ps[6]])
                CP("act", combT[:, t * TT:(t + 1) * TT], psum[6][0:E, :], [b_ps[6]], [combT.b])

            nfb = (dffx + FB - 1) // FB
            seq = [(e_, fb) for e_ in (range(1) if dense else range(E)) for fb in range(nfb)]

            def prefetch(n):
                e_, fb = seq[n]
                wg_, wu_, wd_ = wsets.items[n % 2]
                f0 = fb * FB
                fw = min(FB, dffx - f0)
                if dense:
                    LD(wg_, wg_[:, :, 0:fw], FWg[li][:, f0:f0 + fw].rearrange("(c p) f -> p c f", p=P), reads=[fq_d])
                    LD(wu_, wu_[:, :, 0:fw], FWu[li][:, f0:f0 + fw].rearrange("(c p) f -> p c f", p=P), reads=[fq_d])
                    LD(wd_, wd_[:, 0:fw // P, :], FWd[li][f0:f0 + fw, :].rearrange("(j p) d -> p j d", p=P),
                       reads=[fq_d])
                    return
                sg_, su_, sd_ = mwg[li, e_], mwu[li, e_], mwd[li, e_]
                cpp = max(1, 2048 // fw)
                for dst, src in ((wg_, sg_), (wu_, su_)):
                    sv = src[:, f0:f0 + fw].rearrange("(c p) f -> p c f", p=P)
                    for c0 in range(0, DC, cpp):
                        c1 = min(DC, c0 + cpp)
                        load_cast(dst, dst[:, c0:c1, 0:fw], sv[:, c0:c1, :], (c1 - c0, fw), eng="act")
                dv = sd_[f0:f0 + fw, :].rearrange("(j p) d -> p j d", p=P)
                nj = fw // P
                jpp = max(1, 2048 // D)
                for j0 in range(0, nj, jpp):
                    j1 = min(nj, j0 + jpp)
                    load_cast(wd_, wd_[:, j0:j1, :], dv[:, j0:j1, :], (j1 - j0, D), eng="act")

            def gu_step(n, t):
                e_, fb = seq[n]
                wg_, wu_, wd_ = wsets.items[n % 2]
                f0 = fb * FB
                fw = min(FB, dffx - f0)
                nfc = fw // P
                hs = lambda c, t=t: h_st[:, c, t * TT:(t + 1) * TT]
                if (not dense) and fb == 0:
                    MM(psum[7][:], [(sel[:, e_ * P:(e_ + 1) * P], combT[:, t * TT:(t + 1) * TT])],
                       [sel.b, combT.b], [b_ps[7]])
                    CP("act", cwb[t][:], psum[7][:], [b_ps[7]], [cwb[t].b])
                ab = abr.next()
                for fc in range(nfc):
                    kg = grot.next()
                    ku = urot.next()
                    MM(psum[kg][:], [(wg_[:, c, fc * P:(fc + 1) * P], hs(c)) for c in range(DC)],
                       [wg_.b, h_st.b], [b_ps[kg]])
                    MM(psum[ku][:], [(wu_[:, c, fc * P:(fc + 1) * P], hs(c)) for c in range(DC)],
                       [wu_.b, h_st.b], [b_ps[ku]])
                    sg = sgr.next()
                    ACT(sg[:], psum[kg][:], AF.Silu, [b_ps[kg]], [sg.b])
                    if dense:
                        TTo("dve", ab[:, fc, :], psum[ku][:], sg[:], ALU.mult, [b_ps[ku], sg.b], [ab.b])
                    else:
                        tt_ = ttr.next()
                        TTo("dve", tt_[:], psum[ku][:], sg[:], ALU.mult, [b_ps[ku], sg.b], [tt_.b])
                        TTo("dve", ab[:, fc, :], tt_[:], cwb[t][:], ALU.mult, [tt_.b, cwb[t].b], [ab.b])
                return ab

            def down_step(n, t, ab):
                e_, fb = seq[n]
                wg_, wu_, wd_ = wsets.items[n % 2]
                f0 = fb * FB
                fw = min(FB, dffx - f0)
                nfc = fw // P
                for dc in range(DC):
                    ky = yrot.next()
                    MM(psum[ky][:], [(wd_[:, fc, dc * P:(dc + 1) * P], ab[:, fc, :]) for fc in range(nfc)],
                       [wd_.b, ab.b], [b_ps[ky]])
                    ya = y_acc[:, dc, t * TT:(t + 1) * TT]
                    TTo("dve", ya, psum[ky][:], ya, ALU.add, [b_ps[ky], y_acc.b], [y_acc.b])

            steps = [(n, t) for n in range(len(seq)) for t in range(NSB)]
            prefetch(0)
            if len(seq) > 1:
                prefetch(1)
            abc = gu_step(*steps[0])
            for k, (n, t) in enumerate(steps):
                if k % 6 == 0:
                    bgd_run(bgd_per)
                abn = gu_step(*steps[k + 1]) if k + 1 < len(steps) else None
                down_step(n, t, abc)
                abc = abn
                if t == NSB - 1 and n + 2 < len(seq):
                    prefetch(n + 2)
            for t in range(NSB):
                i = sbi * NSB + t
                xt = xtr.next()
                LD(xt, xt[:], xT_v[:, :, i * TT:(i + 1) * TT], reads=[xT_dram[i]])
                for dc in range(DC):
                    STT("dve", xt[:, dc, :], y_acc[:, dc, t * TT:(t + 1) * TT], gate_f(dc), xt[:, dc, :],
                        ALU.mult, ALU.add, [y_acc.b, md.b, xt.b], [xt.b])
                ST(xt, xt[:], xT_v[:, :, i * TT:(i + 1) * TT], xT_dram[i])
        bgd_run(len(bgd_jobs))
        pg.barrier()
        ar.reset(pbase)
        stage.items = stage.items[:2]

    comb = bool(moe_last and cfg.sparse)
    gb = ar.alloc("gb", [P, D], F32)
    LD(gb, gb[:], fng.partition_broadcast(P))
    xtr = Rot([ar.alloc(f"xt{i}", [P, DC, TT], F32) for i in range(2)])
    otr = Rot([ar.alloc(f"ot{i}", [P, D], F32) for i in range(2)])
    sqj = ar.alloc("sqj", [P, D], F32)
    ssr = Rot([ar.alloc(f"ssr{i}", [P, 8], F32) for i in range(2)])
    if comb:
        mdl = mod[L - 1]
        gate_b = ar.alloc("gate_b", [P, D], F32)
        dgr = Rot([ar.alloc(f"dgt{i}", [P, P], F32) for i in range(2)])
        for c in range(DC):
            dgt = dgr.next()
            TS("dve", dgt[:], ident[:], mdl[:, 5 * DC + c:5 * DC + c + 1], ALU.mult, [ident.b, mdl.b], [dgt.b])
            MM(psum[c // 4][:, (c % 4) * P:(c % 4 + 1) * P], [(ones_f[:], dgt[:])], [ones_f.b, dgt.b], [b_ps[c // 4]])
        CP("act", gate_b[:, 0:512], psum[0][:], [b_ps[0]], [gate_b.b])
        CP("dve", gate_b[:, 512:1024], psum[1][:], [b_ps[1]], [gate_b.b])
        y1r = Rot([ar.alloc(f"y1{i}", [P, D], F32) for i in range(4)])
        y2r = Rot([ar.alloc(f"y2{i}", [P, D], F32) for i in range(4)])
        xsr = Rot([ar.alloc(f"xs{i}", [P, D], F32) for i in range(2)])
    pf_state = {"nblk": 0}

    def pf_A(i, b, xt):
            gbk = i * 4 + b
            o = pf_state["nblk"] % 2
            pf_state["nblk"] += 1
            kb = [4 + 2 * o, 5 + 2 * o]
            ot = otr.next()
            s_ = ssr.next()

            def tp(e, xt=xt, b=b, kb=kb):
                ins = None
                for c in range(DC):
                    bank = kb[c // 4]
                    ins = e.transpose(psum[bank][:, (c % 4) * P:(c % 4 + 1) * P],
                                      xt[:, c, b * P:(b + 1) * P], ident[:])
                return ins
            pg.op("pe", tp, [xt.b, ident.b], [b_ps[kb[0]], b_ps[kb[1]]])
            if comb:
                y1, y2 = pf_gath.pop(gbk)
                xs = xsr.next()
                TS("dve", y1[:], y1[:], w12[:, 2 * gbk:2 * gbk + 1], ALU.mult, [y1.b, w12.b], [y1.b])
                STT("dve", y1[:], y2[:], w12[:, 2 * gbk + 1:2 * gbk + 2], y1[:], ALU.mult, ALU.add,
                    [y1.b, y2.b, w12.b], [y1.b])
                TTo("dve", y1[:], y1[:], gate_b[:], ALU.mult, [y1.b, gate_b.b], [y1.b])
                TTo("dve", xs[:, 0:512], psum[kb[0]][:], y1[:, 0:512], ALU.add, [b_ps[kb[0]], y1.b], [xs.b])
                TTo("dve", xs[:, 512:1024], psum[kb[1]][:], y1[:, 512:1024], ALU.add, [b_ps[kb[1]], y1.b], [xs.b])
                pg.op("act", lambda e, s_=s_, xs=xs: e.activation(out=sqj[:], in_=xs[:], func=AF.Square,
                                                                  accum_out=s_[:, 2:3]),
                      [xs.b], [sqj.b, s_.b])
            else:
                def sqf(e, s_=s_, kb=kb):
                    e.activation(out=sqj[:, 0:512], in_=psum[kb[0]][:], func=AF.Square, accum_out=s_[:, 0:1])
                    return e.activation(out=sqj[:, 512:1024], in_=psum[kb[1]][:], func=AF.Square,
                                        accum_out=s_[:, 1:2])
                pg.op("act", sqf, [b_ps[kb[0]], b_ps[kb[1]]], [sqj.b, s_.b])
                TTo("dve", s_[:, 2:3], s_[:, 0:1], s_[:, 1:2], ALU.add, [s_.b], [s_.b])
            ACT(s_[:, 3:4], s_[:, 2:3], AF.Sqrt, [s_.b, eps_t.b], [s_.b], bias=eps1, scale=1.0 / D)
            return (s_, ot, kb, (xs if comb else None))

    def pf_B(i, b, st):
            s_, ot, kb, xs = st
            RECIP(s_[:, 4:5], s_[:, 3:4], [s_.b], [s_.b])
            if comb:
                STT("dve", ot[:], xs[:], s_[:, 4:5], gb[:], ALU.mult, ALU.mult, [xs.b, s_.b, gb.b], [ot.b])
            else:
                def nrm(e, s_=s_, kb=kb, ot=ot):
                    e.scalar_tensor_tensor(out=ot[:, 0:512], in0=psum[kb[0]][:], scalar=s_[:, 4:5],
                                           in1=gb[:, 0:512], op0=ALU.mult, op1=ALU.mult)
                    return e.scalar_tensor_tensor(out=ot[:, 512:1024], in0=psum[kb[1]][:], scalar=s_[:, 4:5],
                                                  in1=gb[:, 512:1024], op0=ALU.mult, op1=ALU.mult)
                pg.op("dve", nrm, [b_ps[kb[0]], b_ps[kb[1]], s_.b, gb.b], [ot.b])
            r0 = i * TT + b * P
            ev = ST(ot, ot[:], y_out[r0:r0 + P, :], None, q="sp")
            pg.final.append(ev)

    blocks = [(i, b) for i in range(NT) for b in range(4)]
    xts = {}
    pf_gath = {}

    def pf_G(gbk):
        if (not comb) or gbk >= NBLK or gbk in pf_gath:
            return
        y1 = y1r.next()
        y2 = y2r.next()
        for yy, sl_i in ((y1, slot1_i), (y2, slot2_i)):
            pg.dma("pool", [lambda e, yy=yy, sl_i=sl_i, gbk=gbk: e.indirect_dma_start(
                out=yy[:], out_offset=None, in_=Y_slots[:, :],
                in_offset=bass.IndirectOffsetOnAxis(ap=sl_i[:, gbk:gbk + 1], axis=0),
                bounds_check=None)],
                yy.b, reads=[sl_i.b] + yslots_d, writes=[yy.b])
        pf_gath[gbk] = (y1, y2)
    for g_ in range(3):
        pf_G(g_)

    def pf_getxt(i):
        if i not in xts:
            xt = xtr.next()
            LD(xt, xt[:], xT_v[:, :, i * TT:(i + 1) * TT], reads=[xT_dram[i]])
            xts[i] = xt
        return xts[i]
    stA = pf_A(0, 0, pf_getxt(0))
    for n, (i, b) in enumerate(blocks):
        pf_G(n + 3)
        if n + 1 < len(blocks):
            i2, b2 = blocks[n + 1]
            stN = pf_A(i2, b2, pf_getxt(i2))
        else:
            stN = None
        pf_B(i, b, stA)
        stA = stN

    pg.emit()
    return nc


def _consts(S, E):
    NT = S // TT
    inv = np.zeros((2, 3, P, TT), np.float32)
    wins = (2, 4, 8, 16)
    for c in range(2):
        for v, i in enumerate((0, min(1, NT - 1), NT - 1)):
            t = np.arange(i * TT, (i + 1) * TT)
            for g in range(2):
                half = wins[2 * c + g] // 2
                hi = np.clip(t + half, 0, S)
                lo = np.clip(t - half, 0, S)
                inv[c, v, g * 64:(g + 1) * 64, :] = (1.0 / (hi - lo).astype(np.float32))[None, :]
    sel = np.zeros((E, E, P), np.float32)
    for e in range(E):
        sel[e, e, :] = 1.0
    NTL = (2 * S) // TT + E
    utri = np.triu(np.ones((P, P), np.float32), k=1)
    rc = np.zeros((P, NTL + 1), np.float32)
    rc[:, :NTL] = (np.arange(NTL, dtype=np.float32) * TT)[None, :]
    rc[:, NTL] = np.arange(P, dtype=np.float32)
    return {"ident": np.eye(P, dtype=np.float32), "invcnt": inv, "sel": sel.reshape(E, E * P),
            "utri": utri, "rc": rc}


def _pc(v, nchunk):
    v = np.asarray(v, np.float32)
    return np.ascontiguousarray(np.swapaxes(v.reshape(v.shape[:-1] + (nchunk, P)), -1, -2))


def make_in_maps(inputs, cfg):
    f = lambda k: np.ascontiguousarray(np.asarray(inputs[k]), dtype=np.float32)
    x = f("x")
    B = x.shape[0]
    c = f("c")
    L = cfg.L
    shared = {
        "ada_w": f("ada_w"),
        "ada_b_pc": _pc(f("ada_b"), 6 * cfg.DC),
        "mixg_pc": _pc(f("mix_norm_g"), cfg.DC),
        "ffng_pc": _pc(f("ffn_norm_g"), cfg.DC),
        "w_in": f("w_in"),
        "pool_w": f("pool_w"),
        "pool_scale": f("pool_scale"),
        "conv_wT": np.ascontiguousarray(np.swapaxes(f("conv_w"), 1, 2)),
        "conv_b_pc": _pc(f("conv_b"), 3),
        "conv_ln_g_pc": _pc(f("conv_ln_g"), 3),
        "conv_ln_b_pc": _pc(f("conv_ln_b"), 3),
        "sgu_ln_g": f("sgu_ln_g"),
        "sgu_ln_b": f("sgu_ln_b"),
        "sgu_wT": np.ascontiguousarray(np.swapaxes(f("sgu_w"), 2, 3)),
        "sgu_b": f("sgu_b").reshape(L, -1),
        "w_out": f("w_out"),
        "ffn_w_gate": f("ffn_w_gate"),
        "ffn_w_up": f("ffn_w_up"),
        "ffn_w_down": f("ffn_w_down"),
        "router_w": f("router_w"),
        "router_b": f("router_b"),
        "moe_w_gate": f("moe_w_gate"),
        "moe_w_up": f("moe_w_up"),
        "moe_w_down": f("moe_w_down"),
        "final_norm_g": f("final_norm_g"),
    }
    shared.update(_consts(cfg.S, cfg.E))
    maps = []
    for b in range(B):
        m = dict(shared)
        m["x"] = x[b]
        m["c_pc"] = _pc(c[b], cfg.DC)
        maps.append(m)
    return maps


def kernel(**inputs):
    x = np.asarray(inputs["x"])
    B, S, D = x.shape
    cfg = Cfg(S=S, D=D, n_layers=int(np.asarray(inputs["w_in"]).shape[0]),
              d_ff=int(np.asarray(inputs["ffn_w_gate"]).shape[2]),
              n_exp=int(np.asarray(inputs["router_w"]).shape[2]),
              d_ffe=int(np.asarray(inputs["moe_w_gate"]).shape[3]))
    in_maps = make_in_maps(inputs, cfg)
    nc = build(cfg)
    res = run_bass_kernel_spmd(nc, in_maps, core_ids=list(range(B)))
    return np.stack([np.asarray(r["y"], dtype=np.float32) for r in res.results], axis=0)
```
